# Optimizing a Trainium2 kernel written in Bass

```python
import math
import jax, jax.numpy as jnp
from jax import lax
import numpy as np

D_MODEL = 2048
BATCH = 8
SEQ = 4096
DEPTH = 1

CHUNK = 64
GMLP_WIDTH = D_MODEL // 2
GMLP_GROUPS = 8
GMLP_GROUP_DIM = GMLP_WIDTH // GMLP_GROUPS
GMLP_BLOCK = 128
POOL_WIDTH = D_MODEL // 2
POOL_WINDOWS = (2, 4, 8, 16)
POOL_GROUPS = len(POOL_WINDOWS)
POOL_GROUP_DIM = POOL_WIDTH // POOL_GROUPS
SPLIT_U = GMLP_WIDTH
SPLIT_V = 2 * GMLP_WIDTH
SPLIT_P = 2 * GMLP_WIDTH + POOL_WIDTH
SPLIT_GA = SPLIT_P + D_MODEL
PROJ_COLS = SPLIT_GA + D_MODEL
N_EXPERTS = 256
TOP_K = 8
N_GROUPS = 8
TOPK_GROUPS = 4
D_EXPERT = D_MODEL // 4
ROUTED_SCALE = 2.5
EXPERT_BLOCK = 128
LN_EPS = 1e-5
DEEPNORM_ALPHA = (2.0 * DEPTH) ** 0.25
DEEPNORM_BETA = (8.0 * DEPTH) ** -0.25

kernel_name = "hybrid_gmlp_pool_moe_deepnorm_adaln"


def layer_norm(x, gain, bias):
    xf = x.astype(jnp.float32)
    mu = jnp.mean(xf, axis=-1, keepdims=True)
    var = jnp.mean(jnp.square(xf - mu), axis=-1, keepdims=True)
    y = (xf - mu) * lax.rsqrt(var + LN_EPS)
    return (y * gain.astype(jnp.float32) + bias.astype(jnp.float32)).astype(x.dtype)


def gmlp_spatial_gating(u, v, w_s, b_s, g_v, b_v):
    bsz, seq, _ = v.shape
    nb = seq // GMLP_BLOCK
    vg = v.reshape(bsz, seq, GMLP_GROUPS, GMLP_GROUP_DIM)
    vg = layer_norm(vg, g_v.reshape(GMLP_GROUPS, GMLP_GROUP_DIM), b_v.reshape(GMLP_GROUPS, GMLP_GROUP_DIM))
    vb = vg.reshape(bsz, nb, GMLP_BLOCK, GMLP_GROUPS, GMLP_GROUP_DIM)
    pos = jnp.arange(GMLP_BLOCK)
    mask = (pos[None, :] // CHUNK) <= (pos[:, None] // CHUNK)
    w = jnp.where(mask[None], w_s, jnp.zeros((), w_s.dtype))
    s = jnp.einsum('gij,bnjgc->bnigc', w, vb) + jnp.swapaxes(b_s, 0, 1)[:, :, None]
    return u * s.reshape(bsz, seq, GMLP_WIDTH)


def multiscale_pool(p, w_pool, b_pool, ls):
    bsz, seq, _ = p.shape
    pg = p.reshape(bsz, seq, POOL_GROUPS, POOL_GROUP_DIM)
    t = jnp.arange(1, seq + 1, dtype=jnp.float32)
    outs = []
    for g, win in enumerate(POOL_WINDOWS):
        xg = pg[:, :, g, :].astype(jnp.float32)
        cs = jnp.cumsum(xg, axis=1)
        lag = jnp.pad(cs, ((0, 0), (win, 0), (0, 0)))[:, :seq]
        cnt = jnp.minimum(t, float(win))[None, :, None]
        outs.append((cs - lag) / cnt - xg)
    pooled = jnp.stack(outs, axis=2).astype(p.dtype)
    y = jnp.einsum('bsgc,gcd->bsgd', pooled, w_pool) + b_pool.reshape(POOL_GROUPS, POOL_GROUP_DIM)
    return y.reshape(bsz, seq, POOL_WIDTH) * ls


def swiglu(h, wg, wu, wd):
    return (jax.nn.silu(h @ wg) * (h @ wu)) @ wd


def route(h, w_router, b_router):
    n = h.shape[0]
    scores = jax.nn.sigmoid(h.astype(jnp.float32) @ w_router.astype(jnp.float32))
    sel = scores + b_router.astype(jnp.float32)
    gs = sel.reshape(n, N_GROUPS, N_EXPERTS // N_GROUPS)
    group_score = jnp.sum(lax.top_k(gs, 2)[0], axis=-1)
    _, gidx = lax.top_k(group_score, TOPK_GROUPS)
    gmask = jnp.any(gidx[:, :, None] == jnp.arange(N_GROUPS)[None, None, :], axis=1)
    emask = jnp.repeat(gmask, N_EXPERTS // N_GROUPS, axis=1)
    masked = jnp.where(emask, sel, -jnp.inf)
    _, eidx = lax.top_k(masked, TOP_K)
    ew = jnp.take_along_axis(scores, eidx, axis=1)
    ew = ew / jnp.sum(ew, axis=-1, keepdims=True) * ROUTED_SCALE
    return eidx.astype(jnp.int32), ew


def routed_experts(h, eidx, ew, w_gate, w_up, w_down):
    n, d = h.shape
    a = n * TOP_K
    n_blocks = -(-(a + N_EXPERTS * (EXPERT_BLOCK - 1)) // EXPERT_BLOCK)
    p_rows = n_blocks * EXPERT_BLOCK
    flat_e = eidx.reshape(a)
    flat_w = ew.reshape(a)
    order = jnp.argsort(flat_e)
    sorted_e = flat_e[order]
    tok = (order // TOP_K).astype(jnp.int32)
    counts = jnp.bincount(flat_e, length=N_EXPERTS).astype(jnp.int32)
    starts = jnp.cumsum(counts) - counts
    padded = (counts + EXPERT_BLOCK - 1) // EXPERT_BLOCK * EXPERT_BLOCK
    pends = jnp.cumsum(padded)
    pstarts = pends - padded
    dest = pstarts[sorted_e] + (jnp.arange(a, dtype=jnp.int32) - starts[sorted_e])
    row_tok = jnp.full((p_rows,), n, jnp.int32).at[dest].set(tok)
    row_w = jnp.zeros((p_rows,), jnp.float32).at[dest].set(flat_w[order])
    block_start = jnp.arange(n_blocks, dtype=jnp.int32) * EXPERT_BLOCK
    block_e = jnp.minimum(jnp.searchsorted(pends, block_start, side='right'), N_EXPERTS - 1)
    h_pad = jnp.concatenate([h, jnp.zeros((1, d), h.dtype)], axis=0)

    def body(acc, blk):
        e, t, w = blk
        y = swiglu(h_pad[t], w_gate[e], w_up[e], w_down[e]).astype(jnp.float32) * w[:, None]
        return acc.at[t].add(y), None

    acc0 = jnp.zeros((n + 1, d), jnp.float32)
    acc, _ = lax.scan(body, acc0, (block_e.astype(jnp.int32),
                                   row_tok.reshape(n_blocks, EXPERT_BLOCK),
                                   row_w.reshape(n_blocks, EXPERT_BLOCK)))
    return acc[:n].astype(h.dtype)


def setup_inputs(seed: int = 0) -> dict:
    key = jax.random.key(seed)
    ks = jax.random.split(key, 32)
    nrm = lambda k, shape, s: jax.random.normal(k, shape, jnp.float32) * s
    D, L = D_MODEL, DEPTH
    return {
        "x": nrm(ks[0], (BATCH, SEQ, D), 1.0),
        "c": nrm(ks[1], (BATCH, D), 1.0),
        "w_ada": nrm(ks[2], (L, D, 6 * D), 0.5 * D ** -0.5),
        "b_ada": nrm(ks[3], (L, 6 * D), 0.01),
        "w_in": nrm(ks[4], (L, D, PROJ_COLS), D ** -0.5),
        "w_s": nrm(ks[5], (L, GMLP_GROUPS, GMLP_BLOCK, GMLP_BLOCK), GMLP_BLOCK ** -0.5),
        "b_s": 1.0 + nrm(ks[6], (L, GMLP_GROUPS, GMLP_BLOCK), 0.1),
        "g_v": 1.0 + nrm(ks[7], (L, GMLP_WIDTH), 0.01),
        "b_v": nrm(ks[8], (L, GMLP_WIDTH), 0.01),
        "w_pool": nrm(ks[9], (L, POOL_GROUPS, POOL_GROUP_DIM, POOL_GROUP_DIM), POOL_GROUP_DIM ** -0.5),
        "b_pool": nrm(ks[10], (L, POOL_WIDTH), 0.01),
        "ls_pool": 1.0 + nrm(ks[11], (L, POOL_WIDTH), 0.1),
        "w_pa": nrm(ks[12], (L, GMLP_WIDTH, D), GMLP_WIDTH ** -0.5),
        "w_pb": nrm(ks[13], (L, POOL_WIDTH, D), POOL_WIDTH ** -0.5),
        "w_o": nrm(ks[14], (L, D, D), DEEPNORM_BETA * D ** -0.5),
        "ln1_g": 1.0 + nrm(ks[15], (L, D), 0.01),
        "ln1_b": nrm(ks[16], (L, D), 0.01),
        "w_router": nrm(ks[17], (L, D, N_EXPERTS), D ** -0.5),
        "b_router": nrm(ks[18], (L, N_EXPERTS), 0.01),
        "w_gate": nrm(ks[19], (L, N_EXPERTS, D, D_EXPERT), D ** -0.5),
        "w_up": nrm(ks[20], (L, N_EXPERTS, D, D_EXPERT), D ** -0.5),
        "w_down": nrm(ks[21], (L, N_EXPERTS, D_EXPERT, D), DEEPNORM_BETA * D_EXPERT ** -0.5),
        "ws_gate": nrm(ks[22], (L, D, D_EXPERT), D ** -0.5),
        "ws_up": nrm(ks[23], (L, D, D_EXPERT), D ** -0.5),
        "ws_down": nrm(ks[24], (L, D_EXPERT, D), DEEPNORM_BETA * D_EXPERT ** -0.5),
        "ln2_g": 1.0 + nrm(ks[25], (L, D), 0.01),
        "ln2_b": nrm(ks[26], (L, D), 0.01),
    }


def reference(x, c, w_ada, b_ada, w_in, w_s, b_s, g_v, b_v, w_pool, b_pool, ls_pool,
              w_pa, w_pb, w_o, ln1_g, ln1_b, w_router, b_router, w_gate, w_up, w_down,
              ws_gate, ws_up, ws_down, ln2_g, ln2_b):
    bsz, seq, d = x.shape
    for l in range(DEPTH):
        mod = (jax.nn.silu(c) @ w_ada[l] + b_ada[l])[:, None, :]
        sh1, sc1, g1, sh2, sc2, g2 = jnp.split(mod, 6, axis=-1)

        h = x * (1.0 + sc1) + sh1
        z = h @ w_in[l]
        zu, zv, zp, zga, zgb = jnp.split(z, [SPLIT_U, SPLIT_V, SPLIT_P, SPLIT_GA], axis=-1)
        y_a = gmlp_spatial_gating(jax.nn.gelu(zu, approximate=False), jax.nn.gelu(zv, approximate=False),
                                  w_s[l], b_s[l], g_v[l], b_v[l])
        y_b = multiscale_pool(zp, w_pool[l], b_pool[l], ls_pool[l])
        merged = jax.nn.sigmoid(zga) * (y_a @ w_pa[l]) + jax.nn.sigmoid(zgb) * (y_b @ w_pb[l])
        mix = merged @ w_o[l]
        x = layer_norm(DEEPNORM_ALPHA * x + g1 * mix, ln1_g[l], ln1_b[l])

        h = (x * (1.0 + sc2) + sh2).reshape(bsz * seq, d)
        eidx, ew = route(h, w_router[l], b_router[l])
        y = routed_experts(h, eidx, ew, w_gate[l], w_up[l], w_down[l]) + swiglu(h, ws_gate[l], ws_up[l], ws_down[l])
        x = layer_norm(DEEPNORM_ALPHA * x + g2 * y.reshape(bsz, seq, d), ln2_g[l], ln2_b[l])
    return x
```

```python
import numpy as np
from contextlib import ExitStack
import concourse.bass as bass
import concourse.mybir as mybir
from concourse.bass_utils import run_bass_kernel_spmd

F32 = mybir.dt.float32
BF16 = mybir.dt.bfloat16
I32 = mybir.dt.int32
AF = mybir.ActivationFunctionType
ALU = mybir.AluOpType
AX = mybir.AxisListType

D = 2048
T = 4096
ST = 512
NST = T // ST
NTILE = T // 128
NE = 256
NBLK = 510
NSLOT = NBLK * 128
ALPHA = float(2.0 ** 0.25)
EPS = 1e-5
BIG = 1.0e9

_uid = [0]
DEBUG = {}
POOL = {}


def uid():
    _uid[0] += 1
    return _uid[0]


class Sm:
    def __init__(self, h):
        self.h = h
        self.n = 0


class Prog:
    def __init__(self, nc, es):
        self.nc = nc
        self.es = es
        self.q = {k: [] for k in ("pe", "act", "dve", "pool", "sp")}
        self.sm = POOL["eng"][POOL["ei"] % len(POOL["eng"])]
        POOL["ei"] += 1
        self.waited = {k: {} for k in self.q}
        self.dslots = {k: [] for k in self.q}

    def dsem(self):
        sm = POOL["dma"][POOL["di"] % len(POOL["dma"])]
        POOL["di"] += 1
        return sm

    def _waits(self, eng, deps):
        w = self.waited[eng]
        need = {}
        for d in deps:
            if d is None:
                continue
            if isinstance(d, list):
                for dd in d:
                    if dd is None:
                        continue
                    sm, v = dd
                    if w.get(sm, 0) >= v:
                        continue
                    if need.get(sm, 0) < v:
                        need[sm] = v
                continue
            sm, v = d
            if w.get(sm, 0) >= v:
                continue
            if need.get(sm, 0) < v:
                need[sm] = v
        for sm, v in need.items():
            w[sm] = v
        return list(need.items())

    def op(self, eng, fn, deps=()):
        waits = self._waits(eng, deps)
        sm = self.sm[eng]
        sm.n += 1
        c = sm.n

        def run(e, fn=fn, waits=waits, sm=sm):
            for s, v in waits:
                e.wait_ge(s.h, v)
            fn(e).then_inc(sm.h, 1)

        self.q[eng].append(run)
        return (sm, c)

    def dma(self, eng, fn, slot, deps=()):
        waits = self._waits(eng, deps)
        slot.n += 16
        c = slot.n
        if slot not in self.dslots[eng]:
            self.dslots[eng].append(slot)

        def run(e, fn=fn, waits=waits, slot=slot):
            for s, v in waits:
                e.wait_ge(s.h, v)
            fn(e).then_inc(slot.h, 16)

        self.q[eng].append(run)
        return (slot, c)

    def raw(self, eng, fn, deps=()):
        waits = self._waits(eng, deps)

        def run(e, fn=fn, waits=waits):
            for s, v in waits:
                e.wait_ge(s.h, v)
            fn(e)

        self.q[eng].append(run)

    def run(self):
        nc = self.nc
        for eng in self.q:
            for slot in self.dslots[eng]:
                self.raw(eng, lambda e: None, deps=[(slot, slot.n)])
        q = self.q
        with nc.Block() as blk:
            if q["pe"]:
                @blk.tensor
                def _(e):
                    for f in q["pe"]:
                        f(e)
            if q["act"]:
                @blk.scalar
                def _(e):
                    for f in q["act"]:
                        f(e)
            if q["dve"]:
                @blk.vector
                def _(e):
                    for f in q["dve"]:
                        f(e)
            if q["pool"]:
                @blk.gpsimd
                def _(e):
                    for f in q["pool"]:
                        f(e)
            if q["sp"]:
                @blk.sync
                def _(e):
                    for f in q["sp"]:
                        f(e)


def linear_block(nc, name, branches, n_chunks, N, evac, G=4):
    with ExitStack() as es:
        P = Prog(nc, es)
        nb = len(branches)
        rings = []
        for bi, (inT, w, col0, kcb) in enumerate(branches):
            slots = [es.enter_context(nc.sbuf_tensor(f"{name}w{bi}_{s}_{uid()}", [128, kcb, G * 128], BF16)) for s in range(2)]
            sems = [P.dsem() for _ in range(2)]
            rings.append((slots, sems))
        nset = 2
        ps = [[es.enter_context(nc.psum_tensor(f"{name}p{s}_{bi}_{uid()}", [128, 512], F32)) for bi in range(nb)] for s in range(nset)]
        ngroups = (n_chunks + G - 1) // G
        slot_last_read = {}
        ps_free = [None] * nset
        load_t = {}

        def issue_load(g):
            for bi, (inT, w, col0, kcb) in enumerate(branches):
                s = g % 2
                slots, sems = rings[bi]
                ncols = min(G, n_chunks - g * G) * 128
                c0 = col0 + g * G * 128
                src = w[:, c0:c0 + ncols].rearrange("(kc p) c -> p kc c", p=128)
                dst = slots[s][:, :, 0:ncols]
                load_t[(bi, g)] = P.dma("pool", lambda e, dst=dst, src=src: e.dma_start(out=dst, in_=src), sems[s],
                                        deps=[slot_last_read.get((bi, s))])

        issue_load(0)
        if ngroups > 1:
            issue_load(1)
        for c in range(n_chunks):
            g = c // G
            cc = c % G
            s = g % 2
            pset = c % nset
            mm_t = []
            for bi, (inT, w, col0, kcb) in enumerate(branches):
                slots, _ = rings[bi]
                t = None
                for kc in range(kcb):
                    deps = []
                    if kc == 0:
                        deps = [load_t[(bi, g)], ps_free[pset]]
                    t = P.op("pe", lambda e, o=ps[pset][bi][:, 0:N], l=slots[s][:, kc, cc * 128:(cc + 1) * 128], r=inT(kc), a=(kc == 0), b=(kc == kcb - 1):
                             e.matmul(o, lhsT=l, rhs=r, start=a, stop=b), deps)
                mm_t.append(t)
                if cc == G - 1 or c == n_chunks - 1:
                    slot_last_read[(bi, s)] = t
            ps_free[pset] = evac(P, c, [ps[pset][bi][:, 0:N] for bi in range(nb)], mm_t)
            if (cc == G - 1) and g + 2 < ngroups:
                issue_load(g + 2)
        P.run()


def build(stage=99):
    nc = bass.Bass("TRN2", target_bir_lowering=False)

    def din(name, shape, dt=F32):
        return nc.dram_tensor(name, list(shape), dt, kind="ExternalInput").ap()

    def dscr(name, shape, dt=F32):
        return nc.dram_tensor(name, list(shape), dt, kind="Internal").ap()

    x = din("x", [T, D])
    cp_d = din("cp", [128, 16])
    w_ada = din("w_ada", [D, 6 * D])
    badap_d = din("badap", [128, 96])
    w_in = din("w_in", [D, 7168])
    w_s = din("w_s", [8, 128, 128])
    bsT_d = din("bsT", [128, 8, 128])
    gvp_d = din("gvp", [128, 8])
    bvp_d = din("bvp", [128, 8])
    w_pool = din("w_pool", [4, 256, 256])
    bpp_d = din("bpp", [128, 8])
    lsp_d = din("lsp", [128, 8])
    w_pa = din("w_pa", [1024, D])
    w_pb = din("w_pb", [1024, D])
    w_o = din("w_o", [D, D])
    ln1gp_d = din("ln1gp", [128, 16])
    ln1bp_d = din("ln1bp", [128, 16])
    w_router = din("w_router", [D, NE])
    brep_d = din("brep", [128, NE])
    if stage >= 3:
        w_gate = din("w_gate", [NE, D, 512])
        w_up = din("w_up", [NE, D, 512])
        w_down = din("w_down", [NE, 512, D])
    ws_gate = din("ws_gate", [D, 512])
    ws_up = din("ws_up", [D, 512])
    ws_down = din("ws_down", [512, D])
    ln2g_d = din("ln2grep", [128, D])
    ln2b_d = din("ln2brep", [128, D])
    cst_d = din("cst", [128, 5, 128])
    thr_d = din("thr", [128, 512])
    rc_d = din("rc", [128, 4, 16])
    out = nc.dram_tensor("out", [T, D], F32, kind="ExternalOutput").ap()

    xr_d = dscr("xr_d", [T, D])
    h2_d = dscr("h2_d", [T, D], BF16)
    posM_d = dscr("posM_d", [NTILE, 128, NE])
    wn_d = dscr("wn_d", [NTILE, 128, NE])
    xs_d = dscr("xs_d", [NSLOT, D], BF16)
    ys_d = dscr("ys_d", [NSLOT, D], BF16)

    with ExitStack() as gs:
        POOL["eng"] = [{k: Sm(gs.enter_context(nc.semaphore(f"se{i}{k}"))) for k in ("pe", "act", "dve", "pool")} for i in range(8)]
        POOL["dma"] = [Sm(gs.enter_context(nc.semaphore(f"sdm{i}"))) for i in range(64)]
        POOL["ei"] = 0
        POOL["di"] = 0
        for _i in range(DEBUG.get('hog', 0)):
            gs.enter_context(nc.semaphore(f"hog{_i}"))

        def gsb(name, shape, dt=F32):
            return gs.enter_context(nc.sbuf_tensor("g_" + name, list(shape), dt))

        cst = gsb("cst", [128, 5, 128])
        ident_f = cst[:, 0, :]
        ones_f = cst[:, 1, :]
        triS = cst[:, 2, :]
        triI = cst[:, 3, :]
        ident_b = gsb("ident_b", [128, 128], BF16)
        modp = gsb("modp", [128, 96])
        badap = gsb("badap", [128, 96])
        s1p = gsb("s1p", [128, 16])
        A2 = gsb("A2", [128, 16])
        B2 = gsb("B2", [128, 16])
        G1A = gsb("G1A", [128, 16])
        B1A = gsb("B1A", [128, 16])
        ln1gp = gsb("ln1gp", [128, 16])
        ln1bp = gsb("ln1bp", [128, 16])
        gvp = gsb("gvp", [128, 8])
        bvp = gsb("bvp", [128, 8])
        lsp = gsb("lsp", [128, 8])
        blsp = gsb("blsp", [128, 8])
        cpt = gsb("cpt", [128, 16])
        scb = gsb("scb", [128, 16, 1], BF16)
        WmT = gsb("WmT", [128, 8, 128], BF16)
        wpool_sb = gsb("wpool_sb", [128, 4, 2, 256], BF16)
        bsT = gsb("bsT", [128, 8, 128])
        brep = gsb("brep", [128, NE])
        rc_sb = gsb("rc_sb", [128, 4, 16])
        Srun = gsb("Srun", [128, NE])
        slots_all = gsb("slots_all", [128, NTILE, 8], I32)
        W8_all = gsb("W8_all", [128, NTILE, 8])
        idx_all = gsb("idx_all", [128, 4, 512], I32)
        pcol = cst[:, 4, 0:1]
        g1p = modp[:, 32:48]
        g2p = modp[:, 80:96]

        with ExitStack() as es:
            P = Prog(nc, es)
            ws_raw = es.enter_context(nc.sbuf_tensor("ws_raw", [128, 8, 128], F32))
            wp_raw = es.enter_context(nc.sbuf_tensor("wp_raw", [128, 4, 2, 256], F32))
            psw = es.enter_context(nc.psum_tensor("psw", [128, 8, 128], F32))
            sl = P.dsem()
            loads = [(cst[:], cst_d), (badap[:], badap_d), (ln1gp[:], ln1gp_d), (ln1bp[:], ln1bp_d), (gvp[:], gvp_d),
                     (bvp[:], bvp_d), (lsp[:], lsp_d), (blsp[:], bpp_d), (cpt[:], cp_d), (bsT[:], bsT_d), (brep[:], brep_d),
                     (rc_sb[:], rc_d), (ws_raw[:], w_s.rearrange("g i j -> i g j")),
                     (wp_raw[:], w_pool.rearrange("g (kk p) c -> p g kk c", p=128)),
                     ]
            lt = None
            for dst, src in loads:
                lt = P.dma("sp", lambda e, dst=dst, src=src: e.dma_start(out=dst, in_=src), sl)
            t = P.op("act", lambda e: e.activation(out=scb[:, :, 0], in_=cpt[:], func=AF.Silu), [lt])
            t = P.op("dve", lambda e: e.tensor_copy(out=ident_b[:], in_=ident_f), [lt])
            t = P.op("dve", lambda e: e.tensor_tensor(out=blsp[:], in0=blsp[:], in1=lsp[:], op=ALU.mult), [t])
            t = P.op("dve", lambda e: e.tensor_copy(out=wpool_sb[:], in_=wp_raw[:]), [t])
            t = P.op("dve", lambda e: e.memset(Srun[:], 0.0), [t])
            tp = None
            for g in range(8):
                tp = P.op("pe", lambda e, g=g: e.transpose(psw[:, g, :], ws_raw[:, g, :], ident_f), [lt])
            t = P.op("dve", lambda e: e.tensor_copy(out=WmT[:], in_=psw[:]), [tp, t])
            t = P.op("dve", lambda e: e.memset(WmT[64:128, :, 0:64], 0.0), [t])
            P.run()

        def evac_mod(P, c, ps, mm):
            t = P.op("dve", lambda e: e.tensor_tensor(out=modp[:, c:c + 1], in0=ps[0], in1=badap[:, c:c + 1], op=ALU.add), [mm[0]])
            return [t]

        linear_block(nc, "mod", [(lambda kc: scb[:, kc, :], w_ada, 0, 16)], 96, 1, evac_mod, G=4)

        with ExitStack() as es:
            P = Prog(nc, es)
            s2p = es.enter_context(nc.sbuf_tensor("s2p", [128, 16], F32))
            sh1 = modp[:, 0:16]; sc1 = modp[:, 16:32]; sh2 = modp[:, 48:64]; sc2 = modp[:, 64:80]
            t = P.op("dve", lambda e: e.tensor_scalar_add(out=s1p[:], in0=sc1, scalar1=1.0))
            t = P.op("dve", lambda e: e.tensor_scalar_add(out=s2p[:], in0=sc2, scalar1=1.0), [t])
            t = P.op("dve", lambda e: e.tensor_tensor(out=A2[:], in0=ln1gp[:], in1=s2p[:], op=ALU.mult), [t])
            t = P.op("dve", lambda e: e.tensor_tensor(out=B2[:], in0=ln1bp[:], in1=s2p[:], op=ALU.mult), [t])
            t = P.op("dve", lambda e: e.tensor_tensor(out=B2[:], in0=B2[:], in1=sh2, op=ALU.add), [t])
            t = P.op("dve", lambda e: e.tensor_scalar_mul(out=G1A[:], in0=ln1gp[:], scalar1=ALPHA), [t])
            t = P.op("dve", lambda e: e.tensor_scalar_mul(out=B1A[:], in0=ln1bp[:], scalar1=ALPHA), [t])
            P.run()

        with ExitStack() as ms:
            def msb(name, shape, dt=F32):
                return ms.enter_context(nc.sbuf_tensor(name, list(shape), dt))
            xTa = msb("xTa", [128, 16, ST])
            slabH = msb("slabH", [128, 16, ST], BF16)
            slabV1 = msb("slabV1", [128, 8, ST])
            slabV2 = msb("slabV2", [128, 8, ST])
            uT = msb("uT", [128, 8, ST], BF16)
            pT = msb("pT", [128, 8, 16 + ST])
            yaT = msb("yaT", [128, 8, ST], BF16)
            ybT = msb("ybT", [128, 8, ST], BF16)
            hT = slabH
            h2Tb = slabH
            vT = slabV1
            mv = slabV2[:].bitcast(BF16)

            def mergedT(kc):
                return mv[:, kc // 2, (kc % 2) * ST:(kc % 2 + 1) * ST]

            def h2T(kc):
                return slabV1[:, kc, :] if kc < 8 else slabV2[:, kc - 8, :]
            AshT = ybT

            with ExitStack() as es:
                P = Prog(nc, es)
                P.op("dve", lambda e: e.memset(pT[:, :, 0:16], 0.0))
                P.run()

            n_st = DEBUG.get('nst', NST) if stage >= 2 else 1
            for st in range(n_st):
                t0 = st * ST
                LIM = DEBUG.get('lim', 99) if st >= 1 else 99
                with ExitStack() as es:
                    P = Prog(nc, es)
                    xt = [es.enter_context(nc.sbuf_tensor(f"xt{i}_{st}", [128, D], F32)) for i in range(2)]
                    pst = [es.enter_context(nc.psum_tensor(f"pst{i}_{st}", [128, 512], F32)) for i in range(8)]
                    xs = [P.dsem() for _ in range(2)]
                    x_read = [None, None]
                    ps_free = [None] * 8
                    for j in range(4):
                        b = j % 2
                        lt = P.dma("sp", lambda e, j=j, b=b: e.dma_start(out=xt[b][:], in_=x[t0 + j * 128:t0 + (j + 1) * 128, :]), xs[b], deps=[x_read[b]])
                        for q in range(4):
                            bank = (j % 2) * 4 + q
                            tp = None
                            for k4 in range(4):
                                kc = q * 4 + k4
                                tp = P.op("pe", lambda e, bank=bank, k4=k4, kc=kc, b=b: e.transpose(pst[bank][:, k4 * 128:(k4 + 1) * 128], xt[b][:, kc * 128:(kc + 1) * 128], ident_f),
                                          [lt, ps_free[bank]] if k4 == 0 else [])
                            rd = []
                            for k4 in range(4):
                                kc = q * 4 + k4
                                rd.append(P.op("act", lambda e, bank=bank, k4=k4, kc=kc, j=j: e.activation(out=hT[:, kc, j * 128:(j + 1) * 128], in_=pst[bank][:, k4 * 128:(k4 + 1) * 128],
                                                                                                   func=AF.Identity, scale=s1p[:, kc:kc + 1], bias=modp[:, kc:kc + 1]), [tp]))
                            rd.append(P.op("dve", lambda e, bank=bank, q=q, j=j: e.tensor_scalar_mul(out=xTa[:, q * 4:(q + 1) * 4, j * 128:(j + 1) * 128],
                                                                                              in0=pst[bank][:].rearrange("p (a b) -> p a b", a=4), scalar1=ALPHA), [tp]))
                            ps_free[bank] = rd
                            if q == 3:
                                x_read[b] = tp
                    P.run()

                if LIM < 2:
                    break
                def evac_uvp(P, c, ps, mm):
                    if c < 8:
                        t = P.op("act", lambda e: e.activation(out=uT[:, c, :], in_=ps[0], func=AF.Gelu), [mm[0]])
                    elif c < 16:
                        t = P.op("act", lambda e: e.activation(out=vT[:, c - 8, :], in_=ps[0], func=AF.Gelu), [mm[0]])
                    else:
                        t = P.op("dve", lambda e: e.tensor_copy(out=pT[:, c - 16, 16:16 + ST], in_=ps[0]), [mm[0]])
                    return [t]
                linear_block(nc, f"uvp{st}", [(lambda kc: hT[:, kc, :], w_in, 0, 16)], 24, ST, evac_uvp, G=4)

                if LIM < 3:
                    break
                with ExitStack() as es:
                    P = Prog(nc, es)
                    def sb(name, shape, dt=F32):
                        return es.enter_context(nc.sbuf_tensor(f"{name}_{st}", list(shape), dt))
                    sq = [sb(f"sq{i}", [128, ST]) for i in range(2)]
                    mean = [sb(f"mean{i}", [128, ST]) for i in range(2)]
                    var = [sb(f"var{i}", [128, ST]) for i in range(2)]
                    dd = [sb(f"dd{i}", [128, ST]) for i in range(2)]
                    vnT = [sb(f"vnT{i}", [128, ST], BF16) for i in range(2)]
                    vn = [sb(f"vn{i}", [128, 4, 128], BF16) for i in range(2)]
                    tmp = [sb(f"tmp{i}", [128, ST]) for i in range(2)]
                    pa_ = [sb(f"pa{i}", [128, 2, 16 + ST]) for i in range(2)]
                    pooled = sb("pooled", [128, 8, ST], BF16)
                    tmp16 = sb("tmp16", [128, 2, 16])
                    psA = [es.enter_context(nc.psum_tensor(f"psA{i}_{st}", [128, 512], F32)) for i in range(2)]
                    psB = [es.enter_context(nc.psum_tensor(f"psB{i}_{st}", [128, 512], F32)) for i in range(2)]
                    psT = es.enter_context(nc.psum_tensor(f"psT_{st}", [128, 1024], BF16))
                    psS = [es.enter_context(nc.psum_tensor(f"psS{i}_{st}", [128, 512], F32)) for i in range(2)]
                    psY = es.enter_context(nc.psum_tensor(f"psY_{st}", [128, 512], F32))
                    last = {}
                    for g in range(8):
                        b = g % 2
                        t_sq = P.op("act", lambda e, g=g, b=b: e.activation(out=sq[b][:], in_=vT[:, g, :], func=AF.Square), [last.get(("sqr", b))])
                        t_m1 = P.op("pe", lambda e, g=g, b=b: e.matmul(psA[b][:], lhsT=ones_f, rhs=vT[:, g, :], start=True, stop=True), [last.get(("psA", b))])
                        t_m2 = P.op("pe", lambda e, g=g, b=b: e.matmul(psB[b][:], lhsT=ones_f, rhs=sq[b][:], start=True, stop=True), [t_sq, last.get(("psB", b))])
                        last[("sqr", b)] = t_m2
                        t_mean = P.op("dve", lambda e, b=b: e.tensor_scalar_mul(out=mean[b][:], in0=psA[b][:], scalar1=1.0 / 128), [t_m1, last.get(("mean", b))])
                        last[("psA", b)] = t_mean
                        t_msq = P.op("dve", lambda e, b=b: e.tensor_tensor(out=var[b][:], in0=mean[b][:], in1=mean[b][:], op=ALU.mult), [t_mean, last.get(("var", b))])
                        t_var = P.op("dve", lambda e, b=b: e.scalar_tensor_tensor(out=var[b][:], in0=psB[b][:], scalar=1.0 / 128, in1=var[b][:], op0=ALU.mult, op1=ALU.subtract), [t_msq, t_m2])
                        last[("psB", b)] = t_var
                        t_sd = P.op("act", lambda e, b=b: e.activation(out=var[b][:], in_=var[b][:], func=AF.Sqrt, bias=EPS, scale=1.0), [t_var])
                        t_rs = P.op("dve", lambda e, b=b: e.reciprocal(out=var[b][:], in_=var[b][:]), [t_sd])
                        t_d = P.op("dve", lambda e, g=g, b=b: e.tensor_tensor(out=dd[b][:], in0=vT[:, g, :], in1=mean[b][:], op=ALU.subtract), [t_rs, last.get(("dd", b))])
                        last[("mean", b)] = t_d
                        t_d2 = P.op("dve", lambda e, b=b: e.tensor_tensor(out=dd[b][:], in0=dd[b][:], in1=var[b][:], op=ALU.mult), [t_d])
                        last[("var", b)] = t_d2
                        t_vn = P.op("act", lambda e, g=g, b=b: e.activation(out=vnT[b][:], in_=dd[b][:], func=AF.Identity, scale=gvp[:, g:g + 1], bias=bvp[:, g:g + 1]), [t_d2, last.get(("vnT", b))])
                        last[("dd", b)] = t_vn
                        tp = None
                        for j in range(4):
                            tp = P.op("pe", lambda e, j=j, b=b: e.transpose(psT[:, (b * 4 + j) * 128:(b * 4 + j + 1) * 128], vnT[b][:, j * 128:(j + 1) * 128], ident_b[:]),
                                      [t_vn, last.get(("psT", b))] if j == 0 else [])
                        last[("vnT", b)] = tp
                        t_cp = P.op("act", lambda e, b=b: e.activation(out=vn[b][:], in_=psT[:, b * 512:(b + 1) * 512].rearrange("p (a c) -> p a c", a=4), func=AF.Identity), [tp, last.get(("vn", b))])
                        last[("psT", b)] = t_cp
                        ts_ = None
                        for j in range(4):
                            ts_ = P.op("pe", lambda e, j=j, b=b, g=g: e.matmul(psS[b][:, j * 128:(j + 1) * 128], lhsT=vn[b][:, j, :], rhs=WmT[:, g, :], start=True, stop=True),
                                       [t_cp, last.get(("psS", b))] if j == 0 else [])
                        last[("vn", b)] = ts_
                        t_a = P.op("dve", lambda e, b=b, g=g: e.tensor_tensor(out=tmp[b][:].rearrange("p (a c) -> p a c", a=4), in0=psS[b][:].rearrange("p (a c) -> p a c", a=4),
                                                                             in1=bsT[:, g, :].unsqueeze(1).broadcast_to([128, 4, 128]), op=ALU.add), [ts_, last.get(("tmp", b))])
                        last[("psS", b)] = t_a
                        t_y = P.op("dve", lambda e, b=b, g=g: e.tensor_tensor(out=yaT[:, g, :], in0=tmp[b][:], in1=uT[:, g, :], op=ALU.mult), [t_a])
                        last[("tmp", b)] = t_y
                    W = ST + 16
                    t_prev = None
                    t_mm_prev = None
                    for g in range(4):
                        src = pT[:, 2 * g:2 * g + 2, :]
                        cur = src
                        lo = 0
                        tt = t_prev
                        for lvl in range(g + 1):
                            sh = 1 << lvl
                            dst = pa_[lvl % 2]
                            nlo = lo + sh
                            tt = P.op("pool", lambda e, dst=dst, cur=cur, nlo=nlo, sh=sh: e.tensor_tensor(out=dst[:, :, nlo:W], in0=cur[:, :, nlo:W], in1=cur[:, :, nlo - sh:W - sh], op=ALU.add), [tt])
                            cur = dst
                            lo = nlo
                        win = 2 << g
                        tt = P.op("dve", lambda e, cur=cur, g=g, win=win: e.scalar_tensor_tensor(out=pooled[:, 2 * g:2 * g + 2, :], in0=cur[:, :, 16:W], scalar=1.0 / win, in1=pT[:, 2 * g:2 * g + 2, 16:W],
                                                                                              op0=ALU.mult, op1=ALU.subtract), [tt, t_y])
                        if st == 0:
                            tt = P.op("dve", lambda e, cur=cur, g=g: e.tensor_tensor(out=tmp16[:], in0=cur[:, :, 16:32], in1=rc_sb[:, g, :].unsqueeze(1).broadcast_to([128, 2, 16]), op=ALU.mult), [tt])
                            tt = P.op("dve", lambda e, g=g: e.tensor_tensor(out=pooled[:, 2 * g:2 * g + 2, 0:16], in0=tmp16[:], in1=pT[:, 2 * g:2 * g + 2, 16:32], op=ALU.subtract), [tt])
                        t_prev = tt
                        for m in range(2):
                            tm = None
                            for kk in range(2):
                                tm = P.op("pe", lambda e, g=g, m=m, kk=kk: e.matmul(psY[:], lhsT=wpool_sb[:, g, kk, m * 128:(m + 1) * 128], rhs=pooled[:, 2 * g + kk, :], start=(kk == 0), stop=(kk == 1)),
                                          [tt, t_mm_prev] if kk == 0 else [])
                            t_mm_prev = P.op("act", lambda e, g=g, m=m: e.activation(out=ybT[:, 2 * g + m, :], in_=psY[:], func=AF.Identity, scale=lsp[:, 2 * g + m:2 * g + m + 1], bias=blsp[:, 2 * g + m:2 * g + m + 1]), [tm])
                    P.op("pool", lambda e: e.tensor_copy(out=pT[:, :, 0:16], in_=pT[:, :, ST:ST + 16]), [t_prev])
                    P.run()

                if LIM < 4:
                    break
                with ExitStack() as es2:
                    sA = [es2.enter_context(nc.sbuf_tensor(f"sA{i}_{st}", [128, ST], F32)) for i in range(2)]
                    sB = [es2.enter_context(nc.sbuf_tensor(f"sB{i}_{st}", [128, ST], F32)) for i in range(2)]
                    mlast = [None, None]

                    def evac_merge(P, c, ps, mm):
                        b = c % 2
                        t1 = P.op("act", lambda e: e.activation(out=sA[b][:], in_=ps[0], func=AF.Sigmoid), [mm[0], mlast[b]])
                        t2 = P.op("act", lambda e: e.activation(out=sB[b][:], in_=ps[1], func=AF.Sigmoid), [mm[1], mlast[b]])
                        t3 = P.op("dve", lambda e: e.tensor_tensor(out=sA[b][:], in0=ps[2], in1=sA[b][:], op=ALU.mult), [mm[2], t1])
                        t4 = P.op("dve", lambda e: e.tensor_tensor(out=sB[b][:], in0=ps[3], in1=sB[b][:], op=ALU.mult), [mm[3], t2])
                        t5 = P.op("dve", lambda e: e.tensor_tensor(out=mergedT(c), in0=sA[b][:], in1=sB[b][:], op=ALU.add), [t3, t4])
                        mlast[b] = t5
                        return [t1, t2, t3, t4]
                    linear_block(nc, f"mrg{st}", [(lambda kc: hT[:, kc, :], w_in, 3072, 16), (lambda kc: hT[:, kc, :], w_in, 5120, 16),
                                                  (lambda kc: yaT[:, kc, :], w_pa, 0, 8), (lambda kc: ybT[:, kc, :], w_pb, 0, 8)], 16, ST, evac_merge, G=2)

                if LIM < 5:
                    break
                def evac_o(P, c, ps, mm):
                    t = P.op("dve", lambda e: e.scalar_tensor_tensor(out=xTa[:, c, :], in0=ps[0], scalar=g1p[:, c:c + 1], in1=xTa[:, c, :], op0=ALU.mult, op1=ALU.add), [mm[0]])
                    return [t]
                linear_block(nc, f"wo{st}", [(lambda kc: mergedT(kc), w_o, 0, 16)], 16, ST, evac_o, G=4)

                if LIM < 6:
                    break
                with ExitStack() as es:
                    P = Prog(nc, es)
                    def sb(name, shape, dt=F32):
                        return es.enter_context(nc.sbuf_tensor(f"{name}_{st}", list(shape), dt))
                    sq = [sb(f"lsq{i}", [128, ST]) for i in range(2)]
                    mean = sb("lmean", [128, ST])
                    rstd = sb("lrstd", [128, ST])
                    dd = [sb(f"ldd{i}", [128, ST]) for i in range(2)]
                    ps1 = es.enter_context(nc.psum_tensor(f"lps1_{st}", [128, 512], F32))
                    ps2 = es.enter_context(nc.psum_tensor(f"lps2_{st}", [128, 512], F32))
                    m2 = [None, None]
                    t1 = None
                    for kc in range(16):
                        b = kc % 2
                        tq = P.op("act", lambda e, kc=kc, b=b: e.activation(out=sq[b][:], in_=xTa[:, kc, :], func=AF.Square), [m2[b]])
                        t1 = P.op("pe", lambda e, kc=kc: e.matmul(ps1[:], lhsT=ones_f, rhs=xTa[:, kc, :], start=(kc == 0), stop=(kc == 15)))
                        m2[b] = P.op("pe", lambda e, kc=kc, b=b: e.matmul(ps2[:], lhsT=ones_f, rhs=sq[b][:], start=(kc == 0), stop=(kc == 15)), [tq])
                    t = P.op("dve", lambda e: e.tensor_scalar_mul(out=mean[:], in0=ps1[:], scalar1=1.0 / D), [t1, m2[1]])
                    t = P.op("dve", lambda e: e.tensor_tensor(out=rstd[:], in0=mean[:], in1=mean[:], op=ALU.mult), [t])
                    t = P.op("dve", lambda e: e.scalar_tensor_tensor(out=rstd[:], in0=ps2[:], scalar=1.0 / D, in1=rstd[:], op0=ALU.mult, op1=ALU.subtract), [t])
                    t = P.op("act", lambda e: e.activation(out=rstd[:], in_=rstd[:], func=AF.Sqrt, bias=EPS, scale=1.0), [t])
                    t = P.op("dve", lambda e: e.reciprocal(out=rstd[:], in_=rstd[:]), [t])
                    dfree = [None, None]
                    for kc in range(16):
                        b = kc % 2
                        ta = P.op("dve", lambda e, kc=kc, b=b: e.tensor_tensor(out=dd[b][:], in0=xTa[:, kc, :], in1=mean[:], op=ALU.subtract), [t, dfree[b]])
                        tb = P.op("dve", lambda e, b=b: e.tensor_tensor(out=dd[b][:], in0=dd[b][:], in1=rstd[:], op=ALU.mult), [ta])
                        tc1 = P.op("act", lambda e, kc=kc, b=b: e.activation(out=xTa[:, kc, :], in_=dd[b][:], func=AF.Identity, scale=G1A[:, kc:kc + 1], bias=B1A[:, kc:kc + 1]), [tb])
                        tc2 = P.op("act", lambda e, kc=kc, b=b: e.activation(out=h2T(kc), in_=dd[b][:], func=AF.Identity, scale=A2[:, kc:kc + 1], bias=B2[:, kc:kc + 1]), [tb])
                        dfree[b] = [tc1, tc2]
                        P.op("pool", lambda e, kc=kc: e.tensor_copy(out=h2Tb[:, kc, :], in_=h2T(kc)), [tc2])
                    P.run()

                if stage == 1:
                    break

                with ExitStack() as es:
                  if 'router' not in DEBUG.get('skip', ()):
                      P = Prog(nc, es)
                      def sb(name, shape, dt=F32):
                          return es.enter_context(nc.sbuf_tensor(f"{name}_{st}", list(shape), dt))
                      sc = [sb(f"sc{i}", [128, NE]) for i in range(2)]
                      sel = [sb(f"sel{i}", [128, NE]) for i in range(2)]
                      msk = [sb(f"msk{i}", [128, NE]) for i in range(2)]
                      Mt = [sb(f"Mt{i}", [128, NE]) for i in range(2)]
                      Wn = [sb(f"Wn{i}", [128, NE]) for i in range(2)]
                      pM = [sb(f"pM{i}", [128, NE]) for i in range(2)]
                      t8 = [sb(f"t8{i}", [128, 8, 8]) for i in range(2)]
                      gsc = [sb(f"gsc{i}", [128, 8]) for i in range(2)]
                      g8 = [sb(f"g8{i}", [128, 8]) for i in range(2)]
                      pen = [sb(f"pen{i}", [128, 8]) for i in range(2)]
                      v8 = [sb(f"v8{i}", [128, 8]) for i in range(2)]
                      den = [sb(f"den{i}", [128, 1]) for i in range(2)]
                      h2b = [sb(f"h2b{i}", [128, D], BF16) for i in range(2)]
                      psL = [es.enter_context(nc.psum_tensor(f"psL{i}_{st}", [128, 512], F32)) for i in range(2)]
                      psP = [es.enter_context(nc.psum_tensor(f"psP{i}_{st}", [128, 512], F32)) for i in range(2)]
                      psH = es.enter_context(nc.psum_tensor(f"psH_{st}", [128, D], BF16))
                      so = [P.dsem() for _ in range(7)]
                      wr_sb = sb("wr_sb", [128, 16, NE])
                      t_wr = P.dma("sp", lambda e: e.dma_start(out=wr_sb[:], in_=w_router.rearrange("(kc p) e -> p kc e", p=128)), so[6])
                      last = {}
                      t_srun = None
                      for j in range(4):
                          b = j % 2
                          ti = st * 4 + j
                          tl = None
                          for kc in range(16):
                              tl = P.op("pe", lambda e, kc=kc, j=j, b=b: e.matmul(psL[b][:, 0:NE], lhsT=h2T(kc)[:, j * 128:(j + 1) * 128], rhs=wr_sb[:, kc, :], start=(kc == 0), stop=(kc == 15)),
                                        [last.get(("psL", b)), t_wr] if kc == 0 else [])
                          t = P.op("act", lambda e, b=b: e.activation(out=sc[b][:], in_=psL[b][:, 0:NE], func=AF.Sigmoid), [tl, last.get(("sc", b))])
                          last[("psL", b)] = t
                          t = P.op("dve", lambda e, b=b: e.tensor_tensor(out=sel[b][:], in0=sc[b][:], in1=brep[:], op=ALU.add), [t, last.get(("sel", b))])
                          for g in range(8):
                              t = P.op("dve", lambda e, b=b, g=g: e.max(out=t8[b][:, g, :], in_=sel[b][:, g * 32:(g + 1) * 32]), [t])
                          t = P.op("dve", lambda e, b=b: e.tensor_tensor(out=gsc[b][:], in0=t8[b][:, :, 0], in1=t8[b][:, :, 1], op=ALU.add), [t])
                          t = P.op("dve", lambda e, b=b: e.max(out=g8[b][:], in_=gsc[b][:]), [t])
                          t = P.op("dve", lambda e, b=b: e.tensor_scalar(out=pen[b][:], in0=gsc[b][:], scalar1=g8[b][:, 3:4], scalar2=None, op0=ALU.is_ge), [t])
                          t = P.op("dve", lambda e, b=b: e.tensor_scalar(out=pen[b][:], in0=pen[b][:], scalar1=-1.0, scalar2=BIG, op0=ALU.add, op1=ALU.mult), [t])
                          t = P.op("dve", lambda e, b=b: e.tensor_tensor(out=msk[b][:].rearrange("p (g c) -> p g c", g=8), in0=sel[b][:].rearrange("p (g c) -> p g c", g=8),
                                                                       in1=pen[b][:].unsqueeze(2).broadcast_to([128, 8, 32]), op=ALU.add), [t])
                          t = P.op("dve", lambda e, b=b: e.max(out=v8[b][:], in_=msk[b][:]), [t])
                          tM = P.op("dve", lambda e, b=b: e.tensor_scalar(out=Mt[b][:], in0=msk[b][:], scalar1=v8[b][:, 7:8], scalar2=None, op0=ALU.is_ge), [t, last.get(("Mt", b))])
                          last[("sel", b)] = tM
                          t = P.op("dve", lambda e, b=b: e.tensor_tensor(out=Wn[b][:], in0=Mt[b][:], in1=sc[b][:], op=ALU.mult), [tM, last.get(("Wn", b))])
                          last[("sc", b)] = t
                          t = P.op("dve", lambda e, b=b: e.reduce_sum(out=den[b][:], in_=Wn[b][:], axis=AX.X), [t])
                          t = P.op("dve", lambda e, b=b: e.reciprocal(out=den[b][:], in_=den[b][:]), [t])
                          tW = P.op("dve", lambda e, b=b: e.tensor_scalar(out=Wn[b][:], in0=Wn[b][:], scalar1=den[b][:, 0:1], scalar2=2.5, op0=ALU.mult, op1=ALU.mult), [t])
                          last[("Wn", b)] = P.dma("sp", lambda e, b=b, ti=ti: e.dma_start(out=wn_d[ti], in_=Wn[b][:]), so[b], deps=[tW])
                          tp1 = P.op("pe", lambda e, b=b: e.matmul(psP[b][:, 0:NE], lhsT=ones_f, rhs=Srun[:], start=True, stop=False), [t_srun, last.get(("psP", b))])
                          tp2 = P.op("pe", lambda e, b=b: e.matmul(psP[b][:, 0:NE], lhsT=triS, rhs=Mt[b][:], start=False, stop=True), [tM])
                          t_srun = P.op("pool", lambda e, b=b: e.tensor_tensor(out=Srun[:], in0=Srun[:], in1=Mt[b][:], op=ALU.add), [tp2, tM])
                          tpm = P.op("dve", lambda e, b=b: e.scalar_tensor_tensor(out=pM[b][:], in0=psP[b][:, 0:NE], scalar=1.0, in1=Mt[b][:], op0=ALU.add, op1=ALU.mult), [tp2, last.get(("pM", b))])
                          last[("psP", b)] = tpm
                          last[("pM", b)] = P.dma("sp", lambda e, b=b, ti=ti: e.dma_start(out=posM_d[ti], in_=pM[b][:]), so[2 + b], deps=[tpm])
                          last[("Mt", b)] = [tpm, t_srun]
                          tt = None
                          for kc in range(16):
                              tt = P.op("pe", lambda e, kc=kc, j=j: e.transpose(psH[:, kc * 128:(kc + 1) * 128], h2Tb[:, kc, j * 128:(j + 1) * 128], ident_b[:]),
                                        [last.get("psH")] if kc == 0 else [])
                          tcp = P.op("act", lambda e, b=b: e.activation(out=h2b[b][:], in_=psH[:], func=AF.Identity), [tt, last.get(("h2b", b))])
                          last["psH"] = tcp
                          last[("h2b", b)] = P.dma("sp", lambda e, b=b, ti=ti: e.dma_start(out=h2_d[ti * 128:(ti + 1) * 128, :], in_=h2b[b][:]), so[4 + b], deps=[tcp])
                      P.run()

                with ExitStack() as es2:
                    sg = [es2.enter_context(nc.sbuf_tensor(f"sg{i}_{st}", [128, ST], F32)) for i in range(2)]
                    sgl = [None, None]

                    def evac_sh(P, c, ps, mm):
                        b = c % 2
                        t1 = P.op("act", lambda e: e.activation(out=sg[b][:], in_=ps[0], func=AF.Silu), [mm[0], sgl[b]])
                        t2 = P.op("dve", lambda e: e.tensor_tensor(out=AshT[:, c, :], in0=ps[1], in1=sg[b][:], op=ALU.mult), [mm[1], t1])
                        sgl[b] = t2
                        return [t1, t2]
                    linear_block(nc, f"shg{st}", [(lambda kc: h2Tb[:, kc, :], ws_gate, 0, 16), (lambda kc: h2Tb[:, kc, :], ws_up, 0, 16)], 4, ST, evac_sh, G=2)

                def evac_sd(P, c, ps, mm):
                    t = P.op("dve", lambda e: e.scalar_tensor_tensor(out=xTa[:, c, :], in0=ps[0], scalar=g2p[:, c:c + 1], in1=xTa[:, c, :], op0=ALU.mult, op1=ALU.add), [mm[0]])
                    return [t]
                linear_block(nc, f"shd{st}", [(lambda kc: AshT[:, kc, :], ws_down, 0, 4)], 16, ST, evac_sd, G=4)

                emit_tout(nc, xTa, xr_d, t0, ident_f, f"to{st}")

            if stage == 1:
                emit_tout(nc, xTa, out, 0, ident_f, "todbg")
                return nc
        if stage == 2:
            with ExitStack() as es:
                P = Prog(nc, es)
                bt = [es.enter_context(nc.sbuf_tensor(f"cpb{i}", [128, D], F32)) for i in range(2)]
                s_in = [P.dsem() for _ in range(2)]
                s_out = [P.dsem() for _ in range(2)]
                lo_ = [None, None]
                for ti in range(NTILE):
                    b = ti % 2
                    a = P.dma("sp", lambda e, ti=ti, b=b: e.dma_start(out=bt[b][:], in_=xr_d[ti * 128:(ti + 1) * 128, :]), s_in[b], deps=[lo_[b]])
                    lo_[b] = P.dma("sp", lambda e, ti=ti, b=b: e.dma_start(out=out[ti * 128:(ti + 1) * 128, :], in_=bt[b][:]), s_out[b], deps=[a])
                P.run()
            return nc

        emit_moe(nc, locals())
    return nc


def emit_tout(nc, srcT, dst_d, t0, ident_f, name):
    with ExitStack() as es:
        P = Prog(nc, es)
        ob = [es.enter_context(nc.sbuf_tensor(f"{name}ob{i}", [128, D], F32)) for i in range(2)]
        pst = [es.enter_context(nc.psum_tensor(f"{name}ps{i}", [128, 512], F32)) for i in range(8)]
        so = [P.dsem() for _ in range(2)]
        ps_free = [None] * 8
        ob_free = [None, None]
        for j in range(4):
            b = j % 2
            cps = []
            for q in range(4):
                bank = b * 4 + q
                tp = None
                for k4 in range(4):
                    kc = q * 4 + k4
                    tp = P.op("pe", lambda e, bank=bank, k4=k4, kc=kc, j=j: e.transpose(pst[bank][:, k4 * 128:(k4 + 1) * 128], srcT[:, kc, j * 128:(j + 1) * 128], ident_f),
                              [ps_free[bank]] if k4 == 0 else [])
                eng = "act" if q % 2 == 0 else "dve"
                if eng == "act":
                    tcp = P.op("act", lambda e, bank=bank, q=q, b=b: e.activation(out=ob[b][:, q * 512:(q + 1) * 512], in_=pst[bank][:], func=AF.Identity), [tp, ob_free[b]])
                else:
                    tcp = P.op("dve", lambda e, bank=bank, q=q, b=b: e.tensor_copy(out=ob[b][:, q * 512:(q + 1) * 512], in_=pst[bank][:]), [tp, ob_free[b]])
                ps_free[bank] = tcp
                cps.append(tcp)
            ob_free[b] = P.dma("sp", lambda e, j=j, b=b: e.dma_start(out=dst_d[t0 + j * 128:t0 + (j + 1) * 128, :], in_=ob[b][:]), so[b], deps=cps)
        P.run()


def emit_moe(nc, L):
    import types
    V = types.SimpleNamespace(**L)
    ones_f, triS, triI, ident_f, ident_b = V.ones_f, V.triS, V.triI, V.ident_f, V.ident_b
    Srun, slots_all, W8_all, idx_all, pcol = V.Srun, V.slots_all, V.W8_all, V.idx_all, V.pcol
    xs_d, ys_d, xr_d, h2_d, posM_d, wn_d, out = V.xs_d, V.ys_d, V.xr_d, V.h2_d, V.posM_d, V.wn_d, V.out

    with ExitStack() as gs2:
        pstart = gs2.enter_context(nc.sbuf_tensor("pstart", [128, NE], F32))
        with ExitStack() as es:
            P = Prog(nc, es)
            def sb(name, shape, dt=F32):
                return es.enter_context(nc.sbuf_tensor("m0" + name, list(shape), dt))
            cnt_pp = sb("cnt", [128, 2]); nb_pp = sb("nb", [128, 2]); pad_pp = sb("pad", [128, 2]); pend_pp = sb("pend", [128, 2])
            padbc = sb("padbc", [128, 2, 128]); thr_sb = sb("thr", [128, 512]); ind = [sb(f"ind{i}", [128, 512]) for i in range(2)]
            psC = es.enter_context(nc.psum_tensor("m0psC", [128, 512], F32))
            psS = es.enter_context(nc.psum_tensor("m0psS", [128, 512], F32))
            psE = es.enter_context(nc.psum_tensor("m0psE", [128, 512], F32))
            psB = es.enter_context(nc.psum_tensor("m0psB", [128, 512], F32))
            sl = P.dsem()
            lt = P.dma("sp", lambda e: e.dma_start(out=thr_sb[:], in_=V.thr_d), sl)
            t = None
            for c in range(2):
                t = P.op("pe", lambda e, c=c: e.matmul(psC[:, c:c + 1], lhsT=Srun[:, c * 128:(c + 1) * 128], rhs=ones_f[:, 0:1], start=True, stop=True), [t])
            t = P.op("dve", lambda e: e.tensor_copy(out=cnt_pp[:], in_=psC[:, 0:2]), [t])
            t = P.op("dve", lambda e: e.memset(nb_pp[:], 0.0), [t])
            for m in range(32):
                t = P.op("dve", lambda e, m=m: e.scalar_tensor_tensor(out=nb_pp[:], in0=cnt_pp[:], scalar=128.0 * m, in1=nb_pp[:], op0=ALU.is_gt, op1=ALU.add), [t])
            t = P.op("dve", lambda e: e.tensor_scalar_mul(out=pad_pp[:], in0=nb_pp[:], scalar1=128.0), [t])
            for c in range(2):
                t = P.op("dve", lambda e, c=c: e.tensor_copy(out=padbc[:, c, :], in_=pad_pp[:, c:c + 1].broadcast_to([128, 128])), [t])
            tp = P.op("pe", lambda e: e.matmul(psS[:, 0:128], lhsT=padbc[:, 0, :], rhs=triS, start=True, stop=True), [t])
            tp = P.op("pe", lambda e: e.matmul(psS[:, 128:256], lhsT=padbc[:, 0, :], rhs=ones_f, start=True, stop=False), [tp])
            tp = P.op("pe", lambda e: e.matmul(psS[:, 128:256], lhsT=padbc[:, 1, :], rhs=triS, start=False, stop=True), [tp])
            t = P.op("dve", lambda e: e.tensor_copy(out=pstart[:], in_=psS[:, 0:NE]), [tp])
            tp = P.op("pe", lambda e: e.matmul(psE[:, 0:1], lhsT=triI, rhs=pad_pp[:, 0:1], start=True, stop=True), [tp])
            tp = P.op("pe", lambda e: e.matmul(psE[:, 1:2], lhsT=ones_f, rhs=pad_pp[:, 0:1], start=True, stop=False), [tp])
            tp = P.op("pe", lambda e: e.matmul(psE[:, 1:2], lhsT=triI, rhs=pad_pp[:, 1:2], start=False, stop=True), [tp])
            t = P.op("dve", lambda e: e.tensor_copy(out=pend_pp[:], in_=psE[:, 0:2]), [tp, t])
            for c in range(2):
                t = P.op("dve", lambda e, c=c: e.tensor_scalar(out=ind[c][:], in0=thr_sb[:], scalar1=pend_pp[:, c:c + 1], scalar2=None, op0=ALU.is_ge), [t, lt])
            for c in range(2):
                tp = P.op("pe", lambda e, c=c: e.matmul(psB[:, :], lhsT=ones_f, rhs=ind[c][:], start=(c == 0), stop=(c == 1)), [t, tp])
            pc4 = sb("pc4", [128, 4])
            for q in range(4):
                t = P.op("dve", lambda e, q=q: e.tensor_scalar(out=pc4[:, q:q + 1], in0=pcol, scalar1=4.0, scalar2=float(q), op0=ALU.mult, op1=ALU.add), [t])
            for q in range(4):
                t = P.op("dve", lambda e, q=q: e.tensor_scalar(out=idx_all[:, q, :], in0=psB[:, :], scalar1=512.0, scalar2=pc4[:, q:q + 1], op0=ALU.mult, op1=ALU.add), [tp, t])
            P.run()

        for half in range(4):
            with ExitStack() as es:
                P = Prog(nc, es)
                def sb(name, shape, dt=F32):
                    return es.enter_context(nc.sbuf_tensor(f"dp{half}{name}", list(shape), dt))
                pM = [sb(f"pM{i}", [128, NE]) for i in range(2)]
                Wn = [sb(f"Wn{i}", [128, NE]) for i in range(2)]
                hb = [sb(f"hb{i}", [128, D], BF16) for i in range(2)]
                key = [sb(f"key{i}", [128, NE]) for i in range(2)]
                mk = sb("mk", [128, NE]); junk = sb("junk", [128, NE]); s8 = [sb(f"s8{i}", [128, 8]) for i in range(2)]
                sl = [P.dsem() for _ in range(6)]
                ssc = [P.dsem() for _ in range(2)]
                free = {}
                for ii in range(8):
                    i = half * 8 + ii
                    b = ii % 2
                    l1 = P.dma("sp", lambda e, i=i, b=b: e.dma_start(out=pM[b][:], in_=posM_d[i]), sl[b], deps=[free.get(("pM", b))])
                    l2 = P.dma("sp", lambda e, i=i, b=b: e.dma_start(out=Wn[b][:], in_=wn_d[i]), sl[2 + b], deps=[free.get(("Wn", b))])
                    l3 = P.dma("sp", lambda e, i=i, b=b: e.dma_start(out=hb[b][:], in_=h2_d[i * 128:(i + 1) * 128, :]), sl[4 + b], deps=[free.get(("hb", b))])
                    t = P.op("dve", lambda e, b=b: e.tensor_scalar(out=mk[:], in0=pM[b][:], scalar1=0.5, scalar2=None, op0=ALU.is_gt), [l1])
                    t = P.op("dve", lambda e, b=b: e.tensor_tensor(out=key[b][:], in0=pM[b][:], in1=pstart[:], op=ALU.add), [t, free.get(("key", b))])
                    free[("pM", b)] = t
                    t = P.op("dve", lambda e, b=b: e.tensor_tensor(out=key[b][:], in0=key[b][:], in1=mk[:], op=ALU.mult), [t])
                    t = P.op("dve", lambda e, b=b: e.max(out=s8[b][:], in_=key[b][:]), [t, free.get(("s8", b))])
                    tsl = P.op("dve", lambda e, b=b, i=i: e.tensor_scalar(out=slots_all[:, i, :], in0=s8[b][:], scalar1=-1.0, scalar2=None, op0=ALU.add), [t])
                    for k in range(8):
                        t = P.op("dve", lambda e, b=b, i=i, k=k: e.scalar_tensor_tensor(out=junk[:], in0=key[b][:], scalar=s8[b][:, k:k + 1], in1=Wn[b][:], op0=ALU.is_equal, op1=ALU.mult,
                                                                                    accum_out=W8_all[:, i, k:k + 1]), [t, l2])
                    free[("Wn", b)] = t
                    free[("key", b)] = t
                    free[("s8", b)] = t
                    sc_t = None
                    for k in range(8):
                        sc_t = P.dma("pool", lambda e, b=b, i=i, k=k: e.indirect_dma_start(out=xs_d[:, :], out_offset=bass.IndirectOffsetOnAxis(ap=slots_all[:, i, k:k + 1], axis=0),
                                                                                      in_=hb[b][:, :], in_offset=None), ssc[b], deps=[tsl, l3])
                    free[("hb", b)] = sc_t
                P.run()

        BPB = 102
        for cb in range(0 if 'experts' in DEBUG.get('skip', ()) else DEBUG.get('ncb', NBLK // BPB)):
            with ExitStack() as es:
                P = Prog(nc, es)
                def sb(name, shape, dt=F32):
                    return es.enter_context(nc.sbuf_tensor(f"mx{cb}{name}", list(shape), dt))
                wg = [sb(f"wg{i}", [128, 16, 512], BF16) for i in range(2)]
                wu = [sb(f"wu{i}", [128, 16, 512], BF16) for i in range(2)]
                wd = [sb(f"wd{i}", [128, 4, D], BF16) for i in range(2)]
                NSTG = 6
                stg = [sb(f"stg{i}", [128, 2048]) for i in range(NSTG)]
                Xg = [sb(f"Xg{i}", [128, D], BF16) for i in range(2)]
                XgT = [sb(f"XgT{i}", [128, 16, 128], BF16) for i in range(2)]
                sgt = sb("sgt", [128, 512]); Ab = sb("Ab", [128, 512], BF16); AT = sb("AT", [128, 4, 128], BF16)
                Yb = [sb(f"Yb{i}", [128, D], BF16) for i in range(2)]
                psX = es.enter_context(nc.psum_tensor(f"mx{cb}psX", [128, D], BF16))
                psG = es.enter_context(nc.psum_tensor(f"mx{cb}psG", [128, 512], F32))
                psU = es.enter_context(nc.psum_tensor(f"mx{cb}psU", [128, 512], F32))
                psA = es.enter_context(nc.psum_tensor(f"mx{cb}psA", [128, 512], BF16))
                psY = [es.enter_context(nc.psum_tensor(f"mx{cb}psY{i}", [128, 512], F32)) for i in range(3)]
                sstg = [P.dsem() for _ in range(NSTG)]
                sxg = [P.dsem() for _ in range(2)]; syb = [P.dsem() for _ in range(2)]
                fr = {}
                regs = {}
                wgv = V.w_gate.rearrange("e (p q kk) f -> (e p q) (kk f)", q=4, kk=4)
                wuv = V.w_up.rearrange("e (p q kk) f -> (e p q) (kk f)", q=4, kk=4)
                wdv = V.w_down.rearrange("e (p q) d -> (e p q) d", q=4)
                order = [("g", 0), ("u", 0), ("g", 1), ("u", 1), ("g", 2), ("u", 2), ("g", 3), ("u", 3), ("d", 0), ("d", 1), ("d", 2), ("d", 3)]
                total = BPB * 12
                gt = {}
                ct = {}
                stg_free = [None] * NSTG
                sp_ = {"g": 0, "c": 0}

                def emit_gather(i):
                    bl, c = divmod(i, 12)
                    bg = cb * BPB + bl
                    kind, q = order[c]
                    slot = i % NSTG
                    src = {"g": wgv, "u": wuv, "d": wdv}[kind]

                    def f(e, slot=slot, src=src, bg=bg, q=q):
                        if "b" not in regs:
                            regs["b"] = e.alloc_register(f"bndreg{cb}")
                            e.reg_mov(regs["b"], NE * 512 - 1)
                        return e.indirect_dma_start(out=stg[slot][:, :], out_offset=None, in_=src, in_offset=bass.IndirectOffsetOnAxis(ap=idx_all[:, q, bg:bg + 1], axis=0),
                                                    bounds_check=regs["b"], oob_is_err=False)
                    gt[i] = P.dma("pool", f, sstg[slot], deps=[stg_free[slot]])

                def emit_cast(i):
                    bl, c = divmod(i, 12)
                    s = bl % 2
                    kind, q = order[c]
                    slot = i % NSTG
                    if kind == "g":
                        t = P.op("act", lambda e, s=s, q=q, slot=slot: e.activation(out=wg[s][:, 4 * q:4 * q + 4, :], in_=stg[slot][:].rearrange("p (k f) -> p k f", k=4), func=AF.Identity),
                                 [gt[i], fr.get(("wgu", s))])
                    elif kind == "u":
                        t = P.op("dve", lambda e, s=s, q=q, slot=slot: e.tensor_copy(out=wu[s][:, 4 * q:4 * q + 4, :], in_=stg[slot][:].rearrange("p (k f) -> p k f", k=4)),
                                 [gt[i], fr.get(("wgu", s))])
                    elif q % 2 == 0:
                        t = P.op("dve", lambda e, s=s, q=q, slot=slot: e.tensor_copy(out=wd[s][:, q, :], in_=stg[slot][:]), [gt[i], fr.get(("wd", s))])
                    else:
                        t = P.op("act", lambda e, s=s, q=q, slot=slot: e.activation(out=wd[s][:, q, :], in_=stg[slot][:], func=AF.Identity), [gt[i], fr.get(("wd", s))])
                    stg_free[slot] = t
                    ct[i] = t

                def stream_to_cast(c_end):
                    c_end = min(c_end, total)
                    while sp_["c"] < c_end:
                        while sp_["g"] < min(sp_["c"] + NSTG, total):
                            emit_gather(sp_["g"])
                            sp_["g"] += 1
                        emit_cast(sp_["c"])
                        sp_["c"] += 1

                xl = {}

                def issue_x(bl):
                    bg = cb * BPB + bl
                    s = bl % 2
                    xl[bl] = P.dma("sp", lambda e, bg=bg, s=s: e.dma_start(out=Xg[s][:], in_=xs_d[bg * 128:(bg + 1) * 128, :]), sxg[s], deps=[fr.get(("Xg", s))])

                t1c = {}

                def emit_T1(bl):
                    s = bl % 2
                    tp = None
                    for kc in range(16):
                        tp = P.op("pe", lambda e, kc=kc, s=s: e.transpose(psX[:, kc * 128:(kc + 1) * 128], Xg[s][:].rearrange("s (p k) -> s k p", k=16)[:, kc, :], ident_b[:]),
                                  [xl[bl], fr.get("psX")] if kc == 0 else [])
                    fr[("Xg", s)] = tp
                    c1 = P.op("act", lambda e, s=s: e.activation(out=XgT[s][:, 0:8, :], in_=psX[:, 0:1024].rearrange("p (a c) -> p a c", a=8), func=AF.Identity), [tp, fr.get(("XgT", s))])
                    c2 = P.op("dve", lambda e, s=s: e.tensor_copy(out=XgT[s][:, 8:16, :], in_=psX[:, 1024:2048].rearrange("p (a c) -> p a c", a=8)), [tp, fr.get(("XgT", s))])
                    fr["psX"] = [c1, c2]
                    t1c[bl] = [c1, c2]

                issue_x(0)
                issue_x(1)
                emit_T1(0)
                for bl in range(BPB):
                    bg = cb * BPB + bl
                    s = bl % 2
                    stream_to_cast(bl * 12 + 8)
                    tg = None
                    for kc in range(16):
                        d0 = [[ct[bl * 12 + c] for c in range(8)], t1c[bl], fr.get("psGU")] if kc == 0 else []
                        P.op("pe", lambda e, kc=kc, s=s: e.matmul(psG[:], lhsT=XgT[s][:, kc, :], rhs=wg[s][:, kc, :], start=(kc == 0), stop=(kc == 15)), d0)
                        tg = P.op("pe", lambda e, kc=kc, s=s: e.matmul(psU[:], lhsT=XgT[s][:, kc, :], rhs=wu[s][:, kc, :], start=(kc == 0), stop=(kc == 15)))
                    fr[("wgu", s)] = tg
                    stream_to_cast(bl * 12 + 12)
                    fr[("XgT", s)] = tg
                    if bl + 1 < BPB:
                        emit_T1(bl + 1)
                    ta = P.op("act", lambda e: e.activation(out=sgt[:], in_=psG[:], func=AF.Silu), [tg, fr.get("sgt")])
                    tb = P.op("dve", lambda e: e.tensor_tensor(out=Ab[:], in0=psU[:], in1=sgt[:], op=ALU.mult), [ta, fr.get("Ab")])
                    fr["sgt"] = tb
                    fr["psGU"] = tb
                    tp = None
                    for fc in range(4):
                        tp = P.op("pe", lambda e, fc=fc: e.transpose(psA[:, fc * 128:(fc + 1) * 128], Ab[:].rearrange("s (p k) -> s k p", k=4)[:, fc, :], ident_b[:]), [tb, fr.get("psA")] if fc == 0 else [])
                    fr["Ab"] = tp
                    tat = P.op("act", lambda e: e.activation(out=AT[:], in_=psA[:].rearrange("p (a c) -> p a c", a=4), func=AF.Identity), [tp, fr.get("AT")])
                    fr["psA"] = tat
                    ycp = []
                    ty = None
                    for q in range(4):
                        pq = q % 3
                        for fc in range(4):
                            d0 = [tat, [ct[bl * 12 + 8 + c] for c in range(4)], fr.get(("psY", pq))] if fc == 0 else []
                            ty = P.op("pe", lambda e, fc=fc, q=q, pq=pq, s=s: e.matmul(psY[pq][:], lhsT=AT[:, fc, :], rhs=wd[s][:, fc, q * 512:(q + 1) * 512], start=(fc == 0), stop=(fc == 3)), d0)
                        if q % 2 == 0:
                            tc_ = P.op("act", lambda e, q=q, pq=pq, s=s: e.activation(out=Yb[s][:, q * 512:(q + 1) * 512], in_=psY[pq][:], func=AF.Identity), [ty, fr.get(("Yb", s))])
                        else:
                            tc_ = P.op("dve", lambda e, q=q, pq=pq, s=s: e.tensor_copy(out=Yb[s][:, q * 512:(q + 1) * 512], in_=psY[pq][:]), [ty, fr.get(("Yb", s))])
                        fr[("psY", pq)] = tc_
                        ycp.append(tc_)
                    fr["AT"] = ty
                    fr[("wd", s)] = ty
                    fr[("Yb", s)] = P.dma("sp", lambda e, bg=bg, s=s: e.dma_start(out=ys_d[bg * 128:(bg + 1) * 128, :], in_=Yb[s][:]), syb[s], deps=ycp)
                    if bl + 2 < BPB:
                        issue_x(bl + 2)
                P.run()

        for part in range(0 if 'combine' in DEBUG.get('skip', ()) else 4):
            with ExitStack() as es:
                P = Prog(nc, es)
                def sb(name, shape, dt=F32):
                    return es.enter_context(nc.sbuf_tensor(f"cm{part}{name}", list(shape), dt))
                g2rep = sb("g2rep", [128, D]); ln2g = sb("ln2g", [128, D]); ln2b = sb("ln2b", [128, D])
                rk = [sb(f"rk{i}", [128, 128]) for i in range(2)]
                Yg = [[sb(f"Yg{i}_{k}", [128, D], BF16) for k in range(8)] for i in range(2)]
                xr = [sb(f"xr{i}", [128, D]) for i in range(2)]
                acc = sb("acc", [128, D]); sqj = sb("sqj", [128, D]); ob = [sb(f"ob{i}", [128, D]) for i in range(2)]
                st4 = [sb(f"st{i}", [128, 4]) for i in range(2)]
                psg = [es.enter_context(nc.psum_tensor(f"cm{part}psg{i}", [128, 512], F32)) for i in range(4)]
                sl = P.dsem()
                sgk = [[P.dsem() for k in range(8)] for i in range(2)]
                sxr = [P.dsem() for _ in range(2)]; sob = [P.dsem() for _ in range(2)]
                P.dma("sp", lambda e: e.dma_start(out=ln2g[:], in_=V.ln2g_d), sl)
                lt = P.dma("sp", lambda e: e.dma_start(out=ln2b[:], in_=V.ln2b_d), sl)
                mmt = [None, None]
                t = None
                for kc in range(16):
                    t = P.op("dve", lambda e, kc=kc: e.tensor_scalar_mul(out=rk[kc % 2][:], in0=ident_f, scalar1=V.g2p[:, kc:kc + 1]), [t, mmt[kc % 2]])
                    mmt[kc % 2] = P.op("pe", lambda e, kc=kc: e.matmul(psg[kc // 4][:, (kc % 4) * 128:(kc % 4 + 1) * 128], lhsT=ones_f, rhs=rk[kc % 2][:], start=True, stop=True), [t])
                for q in range(4):
                    t = P.op("dve", lambda e, q=q: e.tensor_copy(out=g2rep[:, q * 512:(q + 1) * 512], in_=psg[q][:]), [t, mmt[0], mmt[1]])
                fr = {}
                for ii in range(8):
                    i = part * 8 + ii
                    b = ii % 2
                    gt = []
                    for k in range(8):
                        gt.append(P.dma("pool", lambda e, b=b, i=i, k=k: e.indirect_dma_start(out=Yg[b][k][:, :], out_offset=None, in_=ys_d[:, :],
                                                                                         in_offset=bass.IndirectOffsetOnAxis(ap=slots_all[:, i, k:k + 1], axis=0)), sgk[b][k], deps=[fr.get(("Yg", b))]))
                    lx = P.dma("sp", lambda e, b=b, i=i: e.dma_start(out=xr[b][:], in_=xr_d[i * 128:(i + 1) * 128, :]), sxr[b], deps=[fr.get(("xr", b))])
                    t = P.op("dve", lambda e, b=b, i=i: e.tensor_scalar_mul(out=acc[:], in0=Yg[b][0][:], scalar1=W8_all[:, i, 0:1]), [gt[0], t, fr.get("acc")])
                    for k in range(1, 8):
                        t = P.op("dve", lambda e, b=b, i=i, k=k: e.scalar_tensor_tensor(out=acc[:], in0=Yg[b][k][:], scalar=W8_all[:, i, k:k + 1], in1=acc[:], op0=ALU.mult, op1=ALU.add), [gt[k], t])
                    fr[("Yg", b)] = t
                    t = P.op("dve", lambda e: e.tensor_tensor(out=acc[:], in0=acc[:], in1=g2rep[:], op=ALU.mult), [t])
                    t = P.op("dve", lambda e, b=b: e.tensor_tensor(out=acc[:], in0=acc[:], in1=xr[b][:], op=ALU.add), [t, lx])
                    fr[("xr", b)] = t
                    ta1 = P.op("act", lambda e, b=b: e.activation(out=sqj[:], in_=acc[:], func=AF.Identity, accum_out=st4[b][:, 0:1]), [t, fr.get("sqj"), fr.get(("st", b))])
                    ta2 = P.op("act", lambda e, b=b: e.activation(out=sqj[:], in_=acc[:], func=AF.Square, accum_out=st4[b][:, 1:2]), [ta1])
                    t = P.op("dve", lambda e, b=b: e.tensor_scalar_mul(out=st4[b][:, 0:2], in0=st4[b][:, 0:2], scalar1=1.0 / D), [ta2])
                    t = P.op("dve", lambda e, b=b: e.tensor_tensor(out=st4[b][:, 2:3], in0=st4[b][:, 0:1], in1=st4[b][:, 0:1], op=ALU.mult), [t])
                    t = P.op("dve", lambda e, b=b: e.tensor_tensor(out=st4[b][:, 2:3], in0=st4[b][:, 1:2], in1=st4[b][:, 2:3], op=ALU.subtract), [t])
                    ts = P.op("act", lambda e, b=b: e.activation(out=st4[b][:, 3:4], in_=st4[b][:, 2:3], func=AF.Sqrt, bias=EPS, scale=1.0), [t])
                    fr["sqj"] = ts
                    t = P.op("dve", lambda e, b=b: e.reciprocal(out=st4[b][:, 3:4], in_=st4[b][:, 3:4]), [ts])
                    t = P.op("dve", lambda e, b=b: e.tensor_scalar(out=ob[b][:], in0=acc[:], scalar1=st4[b][:, 0:1], scalar2=st4[b][:, 3:4], op0=ALU.subtract, op1=ALU.mult), [t, fr.get(("ob", b))])
                    fr["acc"] = t
                    fr[("st", b)] = t
                    tq = P.op("pool", lambda e, b=b: e.tensor_tensor(out=ob[b][:], in0=ob[b][:], in1=ln2g[:], op=ALU.mult), [t, lt])
                    tq = P.op("pool", lambda e, b=b: e.tensor_tensor(out=ob[b][:], in0=ob[b][:], in1=ln2b[:], op=ALU.add), [tq])
                    fr[("ob", b)] = P.dma("sp", lambda e, b=b, i=i: e.dma_start(out=out[i * 128:(i + 1) * 128, :], in_=ob[b][:]), sob[b], deps=[tq])
                P.run()


def _pk(v, n):
    return np.ascontiguousarray(np.asarray(v, np.float32).reshape(n, 128).T)


def make_inputs(b, inp):
    f = lambda a: np.ascontiguousarray(np.asarray(a, np.float32))
    m = {}
    m["x"] = f(inp["x"][b])
    m["cp"] = _pk(inp["c"][b], 16)
    m["w_ada"] = f(inp["w_ada"][0])
    m["badap"] = np.ascontiguousarray(np.concatenate([_pk(inp["b_ada"][0][s * D:(s + 1) * D], 16) for s in range(6)], axis=1))
    m["w_in"] = f(inp["w_in"][0])
    m["w_s"] = f(inp["w_s"][0])
    m["bsT"] = np.ascontiguousarray(np.broadcast_to(f(inp["b_s"][0])[None], (128, 8, 128)))
    m["gvp"] = _pk(inp["g_v"][0], 8)
    m["bvp"] = _pk(inp["b_v"][0], 8)
    m["w_pool"] = f(inp["w_pool"][0])
    m["bpp"] = _pk(inp["b_pool"][0], 8)
    m["lsp"] = _pk(inp["ls_pool"][0], 8)
    m["w_pa"] = f(inp["w_pa"][0])
    m["w_pb"] = f(inp["w_pb"][0])
    m["w_o"] = f(inp["w_o"][0])
    m["ln1gp"] = _pk(inp["ln1_g"][0], 16)
    m["ln1bp"] = _pk(inp["ln1_b"][0], 16)
    m["w_router"] = f(inp["w_router"][0])
    m["brep"] = np.ascontiguousarray(np.broadcast_to(f(inp["b_router"][0])[None], (128, NE)))
    m["w_gate"] = f(inp["w_gate"][0])
    m["w_up"] = f(inp["w_up"][0])
    m["w_down"] = f(inp["w_down"][0])
    m["ws_gate"] = f(inp["ws_gate"][0])
    m["ws_up"] = f(inp["ws_up"][0])
    m["ws_down"] = f(inp["ws_down"][0])
    m["ln2grep"] = np.ascontiguousarray(np.broadcast_to(f(inp["ln2_g"][0])[None], (128, D)))
    m["ln2brep"] = np.ascontiguousarray(np.broadcast_to(f(inp["ln2_b"][0])[None], (128, D)))
    cst = np.zeros((128, 5, 128), np.float32)
    cst[:, 0, :] = np.eye(128)
    cst[:, 1, :] = 1.0
    i = np.arange(128)
    cst[:, 2, :] = (i[:, None] < i[None, :])
    cst[:, 3, :] = (i[:, None] <= i[None, :])
    cst[:, 4, :] = i[:, None]
    m["cst"] = cst
    m["thr"] = np.ascontiguousarray(np.broadcast_to((128.0 * np.arange(512, dtype=np.float32))[None], (128, 512)))
    rc = np.zeros((128, 4, 16), np.float32)
    for g in range(4):
        rc[:, g, :] = 1.0 / np.minimum(np.arange(1, 17), 2 << g)
    m["rc"] = rc
    return m


_NC_CACHE = {}


def kernel(**inputs):
    stage = 99
    if stage not in _NC_CACHE:
        _NC_CACHE[stage] = build(stage)
    nc = _NC_CACHE[stage]
    in_maps = [make_inputs(b, inputs) for b in range(8)]
    res = run_bass_kernel_spmd(nc, in_maps, core_ids=list(range(8)))
    return np.stack([np.asarray(r["out"], np.float32) for r in res.results], axis=0)
```

```python
import numpy as np
from contextlib import ExitStack
import concourse.bass as bass
import concourse.mybir as mybir
from concourse.bass_utils import run_bass_kernel_spmd

F32 = mybir.dt.float32
BF16 = mybir.dt.bfloat16
I32 = mybir.dt.int32
AF = mybir.ActivationFunctionType
ALU = mybir.AluOpType
AX = mybir.AxisListType

D = 2048
T = 4096
ST = 512
NST = T // ST
NTILE = T // 128
NE = 256
NBLK = 510
BPB = 51
NCOND = 3
NSLOT = NBLK * 128
ALPHA = float(2.0 ** 0.25)
EPS = 1e-5
BIG = 1.0e9

_uid = [0]
DEBUG = {}
POOL = {}


def uid():
    _uid[0] += 1
    return _uid[0]


class Sm:
    def __init__(self, h):
        self.h = h
        self.n = 0


class Prog:
    def __init__(self, nc, es, cond=None, dedicated=None):
        self.nc = nc
        self.es = es
        self.cond = cond
        self.dedicated = dedicated
        self.q = {k: [] for k in ("pe", "act", "dve", "pool", "sp")}
        if dedicated is not None:
            self.sm = dedicated["eng"]
            self.dd = list(dedicated["dma"])
        else:
            self.sm = POOL["eng"][POOL["ei"] % len(POOL["eng"])]
            POOL["ei"] += 1
        self.waited = {k: {} for k in self.q}
        self.dslots = {k: [] for k in self.q}

    def dsem(self):
        if self.dedicated is not None:
            return self.dd.pop()
        sm = POOL["dma"][POOL["di"] % len(POOL["dma"])]
        POOL["di"] += 1
        return sm

    def _waits(self, eng, deps):
        w = self.waited[eng]
        need = {}
        for d in deps:
            if d is None:
                continue
            if isinstance(d, list):
                for dd in d:
                    if dd is None:
                        continue
                    sm, v = dd
                    if w.get(sm, 0) >= v:
                        continue
                    if need.get(sm, 0) < v:
                        need[sm] = v
                continue
            sm, v = d
            if w.get(sm, 0) >= v:
                continue
            if need.get(sm, 0) < v:
                need[sm] = v
        for sm, v in need.items():
            w[sm] = v
        return list(need.items())

    def op(self, eng, fn, deps=()):
        waits = self._waits(eng, deps)
        sm = self.sm[eng]
        sm.n += 1
        c = sm.n

        def run(e, fn=fn, waits=waits, sm=sm):
            for s, v in waits:
                e.wait_ge(s.h, v)
            fn(e).then_inc(sm.h, 1)

        self.q[eng].append(run)
        return (sm, c)

    def dma(self, eng, fn, slot, deps=()):
        waits = self._waits(eng, deps)
        slot.n += 16
        c = slot.n
        if slot not in self.dslots[eng]:
            self.dslots[eng].append(slot)

        def run(e, fn=fn, waits=waits, slot=slot):
            for s, v in waits:
                e.wait_ge(s.h, v)
            fn(e).then_inc(slot.h, 16)

        self.q[eng].append(run)
        return (slot, c)

    def raw(self, eng, fn, deps=()):
        waits = self._waits(eng, deps)

        def run(e, fn=fn, waits=waits):
            for s, v in waits:
                e.wait_ge(s.h, v)
            fn(e)

        self.q[eng].append(run)

    def run(self):
        nc = self.nc
        for eng in self.q:
            for slot in self.dslots[eng]:
                self.raw(eng, lambda e: None, deps=[(slot, slot.n)])
        q = self.q
        cond = self.cond

        def body(e, fl):
            if cond is None:
                for f in fl:
                    f(e)
            else:
                r_ = e.alloc_register(f"cflag{uid()}")
                e.reg_load(r_, cond)
                v = e.snap(r_, min_val=0, max_val=1)
                with e.If(v):
                    for f in fl:
                        f(e)
        with nc.Block() as blk:
            if q["pe"]:
                @blk.tensor
                def _(e):
                    body(e, q["pe"])
            if q["act"]:
                @blk.scalar
                def _(e):
                    body(e, q["act"])
            if q["dve"]:
                @blk.vector
                def _(e):
                    body(e, q["dve"])
            if q["pool"]:
                @blk.gpsimd
                def _(e):
                    body(e, q["pool"])
            if q["sp"]:
                @blk.sync
                def _(e):
                    body(e, q["sp"])


def linear_block(nc, name, branches, n_chunks, N, evac, G=4):
    with ExitStack() as es:
        P = Prog(nc, es)
        nb = len(branches)
        rings = []
        for bi, (inT, w, col0, kcb) in enumerate(branches):
            slots = [es.enter_context(nc.sbuf_tensor(f"{name}w{bi}_{s}_{uid()}", [128, kcb, G * 128], BF16)) for s in range(2)]
            sems = [P.dsem() for _ in range(2)]
            rings.append((slots, sems))
        nset = 2
        ps = [[es.enter_context(nc.psum_tensor(f"{name}p{s}_{bi}_{uid()}", [128, 512], F32)) for bi in range(nb)] for s in range(nset)]
        ngroups = (n_chunks + G - 1) // G
        slot_last_read = {}
        ps_free = [None] * nset
        load_t = {}

        def issue_load(g):
            for bi, (inT, w, col0, kcb) in enumerate(branches):
                s = g % 2
                slots, sems = rings[bi]
                ncols = min(G, n_chunks - g * G) * 128
                c0 = col0 + g * G * 128
                src = w[:, c0:c0 + ncols].rearrange("(kc p) c -> p kc c", p=128)
                dst = slots[s][:, :, 0:ncols]
                load_t[(bi, g)] = P.dma("pool", lambda e, dst=dst, src=src: e.dma_start(out=dst, in_=src), sems[s],
                                        deps=[slot_last_read.get((bi, s))])

        issue_load(0)
        if ngroups > 1:
            issue_load(1)
        for c in range(n_chunks):
            g = c // G
            cc = c % G
            s = g % 2
            pset = c % nset
            mm_t = []
            for bi, (inT, w, col0, kcb) in enumerate(branches):
                slots, _ = rings[bi]
                t = None
                for kc in range(kcb):
                    deps = []
                    if kc == 0:
                        deps = [load_t[(bi, g)], ps_free[pset]]
                    t = P.op("pe", lambda e, o=ps[pset][bi][:, 0:N], l=slots[s][:, kc, cc * 128:(cc + 1) * 128], r=inT(kc), a=(kc == 0), b=(kc == kcb - 1):
                             e.matmul(o, lhsT=l, rhs=r, start=a, stop=b), deps)
                mm_t.append(t)
                if cc == G - 1 or c == n_chunks - 1:
                    slot_last_read[(bi, s)] = t
            ps_free[pset] = evac(P, c, [ps[pset][bi][:, 0:N] for bi in range(nb)], mm_t)
            if (cc == G - 1) and g + 2 < ngroups:
                issue_load(g + 2)
        P.run()


def build(stage=99):
    nc = bass.Bass("TRN2", target_bir_lowering=False)

    def din(name, shape, dt=F32):
        return nc.dram_tensor(name, list(shape), dt, kind="ExternalInput").ap()

    def dscr(name, shape, dt=F32):
        return nc.dram_tensor(name, list(shape), dt, kind="Internal").ap()

    x = din("x", [T, D])
    cp_d = din("cp", [128, 16])
    w_ada = din("w_ada", [D, 6 * D])
    badap_d = din("badap", [128, 96])
    w_in = din("w_in", [D, 7168])
    w_s = din("w_s", [8, 128, 128])
    bsT_d = din("bsT", [128, 8, 128])
    gvp_d = din("gvp", [128, 8])
    bvp_d = din("bvp", [128, 8])
    w_pool = din("w_pool", [4, 256, 256])
    bpp_d = din("bpp", [128, 8])
    lsp_d = din("lsp", [128, 8])
    w_pa = din("w_pa", [1024, D])
    w_pb = din("w_pb", [1024, D])
    w_o = din("w_o", [D, D])
    ln1gp_d = din("ln1gp", [128, 16])
    ln1bp_d = din("ln1bp", [128, 16])
    w_router = din("w_router", [D, NE])
    brep_d = din("brep", [128, NE])
    if stage >= 3:
        w_gate = din("w_gate", [NE, D, 512])
        w_up = din("w_up", [NE, D, 512])
        w_down = din("w_down", [NE, 512, D])
    ws_gate = din("ws_gate", [D, 512])
    ws_up = din("ws_up", [D, 512])
    ws_down = din("ws_down", [512, D])
    ln2g_d = din("ln2grep", [128, D])
    ln2b_d = din("ln2brep", [128, D])
    cst_d = din("cst", [128, 5, 128])
    thr_d = din("thr", [128, 512])
    rc_d = din("rc", [128, 4, 16])
    out = nc.dram_tensor("out", [T, D], F32, kind="ExternalOutput").ap()

    xr_d = dscr("xr_d", [T, D])
    h2_d = dscr("h2_d", [T, D], BF16)
    posM_d = dscr("posM_d", [NTILE, 128, NE])
    wn_d = dscr("wn_d", [NTILE, 128, NE])
    xs_d = dscr("xs_d", [NSLOT, D], BF16)
    ys_d = dscr("ys_d", [NSLOT, D], BF16)

    with ExitStack() as gs:
        POOL["eng"] = [{k: Sm(gs.enter_context(nc.semaphore(f"se{i}{k}"))) for k in ("pe", "act", "dve", "pool")} for i in range(4)]
        POOL["dma"] = [Sm(gs.enter_context(nc.semaphore(f"sdm{i}"))) for i in range(40)]
        POOL["ded"] = [{"eng": {k: Sm(gs.enter_context(nc.semaphore(f"sc{i}{k}"))) for k in ("pe", "act", "dve", "pool")},
                        "dma": [Sm(gs.enter_context(nc.semaphore(f"scd{i}_{j}"))) for j in range(10)]} for i in range(NCOND)]
        POOL["regs"] = {}
        POOL["ei"] = 0
        POOL["di"] = 0
        for _i in range(DEBUG.get('hog', 0)):
            gs.enter_context(nc.semaphore(f"hog{_i}"))

        def gsb(name, shape, dt=F32):
            return gs.enter_context(nc.sbuf_tensor("g_" + name, list(shape), dt))

        cst = gsb("cst", [128, 5, 128])
        ident_f = cst[:, 0, :]
        ones_f = cst[:, 1, :]
        triS = cst[:, 2, :]
        triI = cst[:, 3, :]
        ident_b = gsb("ident_b", [128, 128], BF16)
        modp = gsb("modp", [128, 96])
        badap = gsb("badap", [128, 96])
        s1p = gsb("s1p", [128, 16])
        A2 = gsb("A2", [128, 16])
        B2 = gsb("B2", [128, 16])
        G1A = gsb("G1A", [128, 16])
        B1A = gsb("B1A", [128, 16])
        ln1gp = gsb("ln1gp", [128, 16])
        ln1bp = gsb("ln1bp", [128, 16])
        gvp = gsb("gvp", [128, 8])
        bvp = gsb("bvp", [128, 8])
        lsp = gsb("lsp", [128, 8])
        blsp = gsb("blsp", [128, 8])
        cpt = gsb("cpt", [128, 16])
        scb = gsb("scb", [128, 16, 1], BF16)
        WmT = gsb("WmT", [128, 8, 128], BF16)
        wpool_sb = gsb("wpool_sb", [128, 4, 2, 256], BF16)
        bsT = gsb("bsT", [128, 8, 128])
        brep = gsb("brep", [128, NE])
        rc_sb = gsb("rc_sb", [128, 4, 16])
        Srun = gsb("Srun", [128, NE])
        slots_all = gsb("slots_all", [128, NTILE, 8], I32)
        W8_all = gsb("W8_all", [128, NTILE, 8])
        idx_all = gsb("idx_all", [128, 4, 512], I32)
        blkflag = gsb("blkflag", [1, 16], I32)
        pcol = cst[:, 4, 0:1]
        g1p = modp[:, 32:48]
        g2p = modp[:, 80:96]

        with ExitStack() as es:
            P = Prog(nc, es)
            ws_raw = es.enter_context(nc.sbuf_tensor("ws_raw", [128, 8, 128], F32))
            wp_raw = es.enter_context(nc.sbuf_tensor("wp_raw", [128, 4, 2, 256], F32))
            psw = es.enter_context(nc.psum_tensor("psw", [128, 8, 128], F32))
            sl = P.dsem()
            loads = [(cst[:], cst_d), (badap[:], badap_d), (ln1gp[:], ln1gp_d), (ln1bp[:], ln1bp_d), (gvp[:], gvp_d),
                     (bvp[:], bvp_d), (lsp[:], lsp_d), (blsp[:], bpp_d), (cpt[:], cp_d), (bsT[:], bsT_d), (brep[:], brep_d),
                     (rc_sb[:], rc_d), (ws_raw[:], w_s.rearrange("g i j -> i g j")),
                     (wp_raw[:], w_pool.rearrange("g (kk p) c -> p g kk c", p=128)),
                     ]
            lt = None
            for dst, src in loads:
                lt = P.dma("sp", lambda e, dst=dst, src=src: e.dma_start(out=dst, in_=src), sl)
            t = P.op("act", lambda e: e.activation(out=scb[:, :, 0], in_=cpt[:], func=AF.Silu), [lt])
            t = P.op("dve", lambda e: e.tensor_copy(out=ident_b[:], in_=ident_f), [lt])
            t = P.op("dve", lambda e: e.tensor_tensor(out=blsp[:], in0=blsp[:], in1=lsp[:], op=ALU.mult), [t])
            t = P.op("dve", lambda e: e.tensor_copy(out=wpool_sb[:], in_=wp_raw[:]), [t])
            t = P.op("dve", lambda e: e.memset(Srun[:], 0.0), [t])
            tp = None
            for g in range(8):
                tp = P.op("pe", lambda e, g=g: e.transpose(psw[:, g, :], ws_raw[:, g, :], ident_f), [lt])
            t = P.op("dve", lambda e: e.tensor_copy(out=WmT[:], in_=psw[:]), [tp, t])
            t = P.op("dve", lambda e: e.memset(WmT[64:128, :, 0:64], 0.0), [t])
            P.run()

        def evac_mod(P, c, ps, mm):
            t = P.op("dve", lambda e: e.tensor_tensor(out=modp[:, c:c + 1], in0=ps[0], in1=badap[:, c:c + 1], op=ALU.add), [mm[0]])
            return [t]

        linear_block(nc, "mod", [(lambda kc: scb[:, kc, :], w_ada, 0, 16)], 96, 1, evac_mod, G=4)

        with ExitStack() as es:
            P = Prog(nc, es)
            s2p = es.enter_context(nc.sbuf_tensor("s2p", [128, 16], F32))
            sh1 = modp[:, 0:16]; sc1 = modp[:, 16:32]; sh2 = modp[:, 48:64]; sc2 = modp[:, 64:80]
            t = P.op("dve", lambda e: e.tensor_scalar_add(out=s1p[:], in0=sc1, scalar1=1.0))
            t = P.op("dve", lambda e: e.tensor_scalar_add(out=s2p[:], in0=sc2, scalar1=1.0), [t])
            t = P.op("dve", lambda e: e.tensor_tensor(out=A2[:], in0=ln1gp[:], in1=s2p[:], op=ALU.mult), [t])
            t = P.op("dve", lambda e: e.tensor_tensor(out=B2[:], in0=ln1bp[:], in1=s2p[:], op=ALU.mult), [t])
            t = P.op("dve", lambda e: e.tensor_tensor(out=B2[:], in0=B2[:], in1=sh2, op=ALU.add), [t])
            t = P.op("dve", lambda e: e.tensor_scalar_mul(out=G1A[:], in0=ln1gp[:], scalar1=ALPHA), [t])
            t = P.op("dve", lambda e: e.tensor_scalar_mul(out=B1A[:], in0=ln1bp[:], scalar1=ALPHA), [t])
            P.run()

        with ExitStack() as ms:
            def msb(name, shape, dt=F32):
                return ms.enter_context(nc.sbuf_tensor(name, list(shape), dt))
            xTa = msb("xTa", [128, 16, ST])
            slabH = msb("slabH", [128, 16, ST], BF16)
            slabV1 = msb("slabV1", [128, 8, ST])
            slabV2 = msb("slabV2", [128, 8, ST])
            uT = msb("uT", [128, 8, ST], BF16)
            pT = msb("pT", [128, 8, 16 + ST])
            yaT = msb("yaT", [128, 8, ST], BF16)
            ybT = msb("ybT", [128, 8, ST], BF16)
            hT = slabH
            h2Tb = slabH
            vT = slabV1
            mv = slabV2[:].bitcast(BF16)

            def mergedT(kc):
                return mv[:, kc // 2, (kc % 2) * ST:(kc % 2 + 1) * ST]

            def h2T(kc):
                return slabV1[:, kc, :] if kc < 8 else slabV2[:, kc - 8, :]
            AshT = ybT

            with ExitStack() as es:
                P = Prog(nc, es)
                P.op("dve", lambda e: e.memset(pT[:, :, 0:16], 0.0))
                P.run()

            n_st = DEBUG.get('nst', NST) if stage >= 2 else 1
            for st in range(n_st):
                t0 = st * ST
                LIM = DEBUG.get('lim', 99) if st >= 1 else 99
                with ExitStack() as es:
                    P = Prog(nc, es)
                    xt = [es.enter_context(nc.sbuf_tensor(f"xt{i}_{st}", [128, D], F32)) for i in range(2)]
                    pst = [es.enter_context(nc.psum_tensor(f"pst{i}_{st}", [128, 512], F32)) for i in range(8)]
                    xs = [P.dsem() for _ in range(2)]
                    x_read = [None, None]
                    ps_free = [None] * 8
                    for j in range(4):
                        b = j % 2
                        lt = P.dma("sp", lambda e, j=j, b=b: e.dma_start(out=xt[b][:], in_=x[t0 + j * 128:t0 + (j + 1) * 128, :]), xs[b], deps=[x_read[b]])
                        for q in range(4):
                            bank = (j % 2) * 4 + q
                            tp = None
                            for k4 in range(4):
                                kc = q * 4 + k4
                                tp = P.op("pe", lambda e, bank=bank, k4=k4, kc=kc, b=b: e.transpose(pst[bank][:, k4 * 128:(k4 + 1) * 128], xt[b][:, kc * 128:(kc + 1) * 128], ident_f),
                                          [lt, ps_free[bank]] if k4 == 0 else [])
                            rd = []
                            for k4 in range(4):
                                kc = q * 4 + k4
                                rd.append(P.op("act", lambda e, bank=bank, k4=k4, kc=kc, j=j: e.activation(out=hT[:, kc, j * 128:(j + 1) * 128], in_=pst[bank][:, k4 * 128:(k4 + 1) * 128],
                                                                                                   func=AF.Identity, scale=s1p[:, kc:kc + 1], bias=modp[:, kc:kc + 1]), [tp]))
                            rd.append(P.op("dve", lambda e, bank=bank, q=q, j=j: e.tensor_scalar_mul(out=xTa[:, q * 4:(q + 1) * 4, j * 128:(j + 1) * 128],
                                                                                              in0=pst[bank][:].rearrange("p (a b) -> p a b", a=4), scalar1=ALPHA), [tp]))
                            ps_free[bank] = rd
                            if q == 3:
                                x_read[b] = tp
                    P.run()

                if LIM < 2:
                    break
                def evac_uvp(P, c, ps, mm):
                    if c < 8:
                        t = P.op("act", lambda e: e.activation(out=uT[:, c, :], in_=ps[0], func=AF.Gelu), [mm[0]])
                    elif c < 16:
                        t = P.op("act", lambda e: e.activation(out=vT[:, c - 8, :], in_=ps[0], func=AF.Gelu), [mm[0]])
                    else:
                        t = P.op("dve", lambda e: e.tensor_copy(out=pT[:, c - 16, 16:16 + ST], in_=ps[0]), [mm[0]])
                    return [t]
                linear_block(nc, f"uvp{st}", [(lambda kc: hT[:, kc, :], w_in, 0, 16)], 24, ST, evac_uvp, G=4)

                if LIM < 3:
                    break
                with ExitStack() as es:
                    P = Prog(nc, es)
                    def sb(name, shape, dt=F32):
                        return es.enter_context(nc.sbuf_tensor(f"{name}_{st}", list(shape), dt))
                    sq = [sb(f"sq{i}", [128, ST]) for i in range(2)]
                    mean = [sb(f"mean{i}", [128, ST]) for i in range(2)]
                    var = [sb(f"var{i}", [128, ST]) for i in range(2)]
                    dd = [sb(f"dd{i}", [128, ST]) for i in range(2)]
                    vnT = [sb(f"vnT{i}", [128, ST], BF16) for i in range(2)]
                    vn = [sb(f"vn{i}", [128, 4, 128], BF16) for i in range(2)]
                    tmp = [sb(f"tmp{i}", [128, ST]) for i in range(2)]
                    pa_ = [sb(f"pa{i}", [128, 2, 16 + ST]) for i in range(2)]
                    pooled = sb("pooled", [128, 8, ST], BF16)
                    tmp16 = sb("tmp16", [128, 2, 16])
                    psA = [es.enter_context(nc.psum_tensor(f"psA{i}_{st}", [128, 512], F32)) for i in range(2)]
                    psB = [es.enter_context(nc.psum_tensor(f"psB{i}_{st}", [128, 512], F32)) for i in range(2)]
                    psT = es.enter_context(nc.psum_tensor(f"psT_{st}", [128, 1024], BF16))
                    psS = [es.enter_context(nc.psum_tensor(f"psS{i}_{st}", [128, 512], F32)) for i in range(2)]
                    psY = es.enter_context(nc.psum_tensor(f"psY_{st}", [128, 512], F32))
                    last = {}
                    for g in range(8):
                        b = g % 2
                        t_sq = P.op("act", lambda e, g=g, b=b: e.activation(out=sq[b][:], in_=vT[:, g, :], func=AF.Square), [last.get(("sqr", b))])
                        t_m1 = P.op("pe", lambda e, g=g, b=b: e.matmul(psA[b][:], lhsT=ones_f, rhs=vT[:, g, :], start=True, stop=True), [last.get(("psA", b))])
                        t_m2 = P.op("pe", lambda e, g=g, b=b: e.matmul(psB[b][:], lhsT=ones_f, rhs=sq[b][:], start=True, stop=True), [t_sq, last.get(("psB", b))])
                        last[("sqr", b)] = t_m2
                        t_mean = P.op("dve", lambda e, b=b: e.tensor_scalar_mul(out=mean[b][:], in0=psA[b][:], scalar1=1.0 / 128), [t_m1, last.get(("mean", b))])
                        last[("psA", b)] = t_mean
                        t_msq = P.op("dve", lambda e, b=b: e.tensor_tensor(out=var[b][:], in0=mean[b][:], in1=mean[b][:], op=ALU.mult), [t_mean, last.get(("var", b))])
                        t_var = P.op("dve", lambda e, b=b: e.scalar_tensor_tensor(out=var[b][:], in0=psB[b][:], scalar=1.0 / 128, in1=var[b][:], op0=ALU.mult, op1=ALU.subtract), [t_msq, t_m2])
                        last[("psB", b)] = t_var
                        t_sd = P.op("act", lambda e, b=b: e.activation(out=var[b][:], in_=var[b][:], func=AF.Sqrt, bias=EPS, scale=1.0), [t_var])
                        t_rs = P.op("dve", lambda e, b=b: e.reciprocal(out=var[b][:], in_=var[b][:]), [t_sd])
                        t_d = P.op("dve", lambda e, g=g, b=b: e.tensor_tensor(out=dd[b][:], in0=vT[:, g, :], in1=mean[b][:], op=ALU.subtract), [t_rs, last.get(("dd", b))])
                        last[("mean", b)] = t_d
                        t_d2 = P.op("dve", lambda e, b=b: e.tensor_tensor(out=dd[b][:], in0=dd[b][:], in1=var[b][:], op=ALU.mult), [t_d])
                        last[("var", b)] = t_d2
                        t_vn = P.op("act", lambda e, g=g, b=b: e.activation(out=vnT[b][:], in_=dd[b][:], func=AF.Identity, scale=gvp[:, g:g + 1], bias=bvp[:, g:g + 1]), [t_d2, last.get(("vnT", b))])
                        last[("dd", b)] = t_vn
                        tp = None
                        for j in range(4):
                            tp = P.op("pe", lambda e, j=j, b=b: e.transpose(psT[:, (b * 4 + j) * 128:(b * 4 + j + 1) * 128], vnT[b][:, j * 128:(j + 1) * 128], ident_b[:]),
                                      [t_vn, last.get(("psT", b))] if j == 0 else [])
                        last[("vnT", b)] = tp
                        t_cp = P.op("act", lambda e, b=b: e.activation(out=vn[b][:], in_=psT[:, b * 512:(b + 1) * 512].rearrange("p (a c) -> p a c", a=4), func=AF.Identity), [tp, last.get(("vn", b))])
                        last[("psT", b)] = t_cp
                        ts_ = None
                        for j in range(4):
                            ts_ = P.op("pe", lambda e, j=j, b=b, g=g: e.matmul(psS[b][:, j * 128:(j + 1) * 128], lhsT=vn[b][:, j, :], rhs=WmT[:, g, :], start=True, stop=True),
                                       [t_cp, last.get(("psS", b))] if j == 0 else [])
                        last[("vn", b)] = ts_
                        t_a = P.op("dve", lambda e, b=b, g=g: e.tensor_tensor(out=tmp[b][:].rearrange("p (a c) -> p a c", a=4), in0=psS[b][:].rearrange("p (a c) -> p a c", a=4),
                                                                             in1=bsT[:, g, :].unsqueeze(1).broadcast_to([128, 4, 128]), op=ALU.add), [ts_, last.get(("tmp", b))])
                        last[("psS", b)] = t_a
                        t_y = P.op("dve", lambda e, b=b, g=g: e.tensor_tensor(out=yaT[:, g, :], in0=tmp[b][:], in1=uT[:, g, :], op=ALU.mult), [t_a])
                        last[("tmp", b)] = t_y
                    W = ST + 16
                    t_prev = None
                    t_mm_prev = None
                    for g in range(4):
                        src = pT[:, 2 * g:2 * g + 2, :]
                        cur = src
                        lo = 0
                        tt = t_prev
                        for lvl in range(g + 1):
                            sh = 1 << lvl
                            dst = pa_[lvl % 2]
                            nlo = lo + sh
                            tt = P.op("pool", lambda e, dst=dst, cur=cur, nlo=nlo, sh=sh: e.tensor_tensor(out=dst[:, :, nlo:W], in0=cur[:, :, nlo:W], in1=cur[:, :, nlo - sh:W - sh], op=ALU.add), [tt])
                            cur = dst
                            lo = nlo
                        win = 2 << g
                        tt = P.op("dve", lambda e, cur=cur, g=g, win=win: e.scalar_tensor_tensor(out=pooled[:, 2 * g:2 * g + 2, :], in0=cur[:, :, 16:W], scalar=1.0 / win, in1=pT[:, 2 * g:2 * g + 2, 16:W],
                                                                                              op0=ALU.mult, op1=ALU.subtract), [tt, t_y])
                        if st == 0:
                            tt = P.op("dve", lambda e, cur=cur, g=g: e.tensor_tensor(out=tmp16[:], in0=cur[:, :, 16:32], in1=rc_sb[:, g, :].unsqueeze(1).broadcast_to([128, 2, 16]), op=ALU.mult), [tt])
                            tt = P.op("dve", lambda e, g=g: e.tensor_tensor(out=pooled[:, 2 * g:2 * g + 2, 0:16], in0=tmp16[:], in1=pT[:, 2 * g:2 * g + 2, 16:32], op=ALU.subtract), [tt])
                        t_prev = tt
                        for m in range(2):
                            tm = None
                            for kk in range(2):
                                tm = P.op("pe", lambda e, g=g, m=m, kk=kk: e.matmul(psY[:], lhsT=wpool_sb[:, g, kk, m * 128:(m + 1) * 128], rhs=pooled[:, 2 * g + kk, :], start=(kk == 0), stop=(kk == 1)),
                                          [tt, t_mm_prev] if kk == 0 else [])
                            t_mm_prev = P.op("act", lambda e, g=g, m=m: e.activation(out=ybT[:, 2 * g + m, :], in_=psY[:], func=AF.Identity, scale=lsp[:, 2 * g + m:2 * g + m + 1], bias=blsp[:, 2 * g + m:2 * g + m + 1]), [tm])
                    P.op("pool", lambda e: e.tensor_copy(out=pT[:, :, 0:16], in_=pT[:, :, ST:ST + 16]), [t_prev])
                    P.run()

                if LIM < 4:
                    break
                with ExitStack() as es2:
                    sA = [es2.enter_context(nc.sbuf_tensor(f"sA{i}_{st}", [128, ST], F32)) for i in range(2)]
                    sB = [es2.enter_context(nc.sbuf_tensor(f"sB{i}_{st}", [128, ST], F32)) for i in range(2)]
                    mlast = [None, None]

                    def evac_merge(P, c, ps, mm):
                        b = c % 2
                        t1 = P.op("act", lambda e: e.activation(out=sA[b][:], in_=ps[0], func=AF.Sigmoid), [mm[0], mlast[b]])
                        t2 = P.op("act", lambda e: e.activation(out=sB[b][:], in_=ps[1], func=AF.Sigmoid), [mm[1], mlast[b]])
                        t3 = P.op("dve", lambda e: e.tensor_tensor(out=sA[b][:], in0=ps[2], in1=sA[b][:], op=ALU.mult), [mm[2], t1])
                        t4 = P.op("dve", lambda e: e.tensor_tensor(out=sB[b][:], in0=ps[3], in1=sB[b][:], op=ALU.mult), [mm[3], t2])
                        t5 = P.op("dve", lambda e: e.tensor_tensor(out=mergedT(c), in0=sA[b][:], in1=sB[b][:], op=ALU.add), [t3, t4])
                        mlast[b] = t5
                        return [t1, t2, t3, t4]
                    linear_block(nc, f"mrg{st}", [(lambda kc: hT[:, kc, :], w_in, 3072, 16), (lambda kc: hT[:, kc, :], w_in, 5120, 16),
                                                  (lambda kc: yaT[:, kc, :], w_pa, 0, 8), (lambda kc: ybT[:, kc, :], w_pb, 0, 8)], 16, ST, evac_merge, G=2)

                if LIM < 5:
                    break
                def evac_o(P, c, ps, mm):
                    t = P.op("dve", lambda e: e.scalar_tensor_tensor(out=xTa[:, c, :], in0=ps[0], scalar=g1p[:, c:c + 1], in1=xTa[:, c, :], op0=ALU.mult, op1=ALU.add), [mm[0]])
                    return [t]
                linear_block(nc, f"wo{st}", [(lambda kc: mergedT(kc), w_o, 0, 16)], 16, ST, evac_o, G=4)

                if LIM < 6:
                    break
                with ExitStack() as es:
                    P = Prog(nc, es)
                    def sb(name, shape, dt=F32):
                        return es.enter_context(nc.sbuf_tensor(f"{name}_{st}", list(shape), dt))
                    sq = [sb(f"lsq{i}", [128, ST]) for i in range(2)]
                    mean = sb("lmean", [128, ST])
                    rstd = sb("lrstd", [128, ST])
                    dd = [sb(f"ldd{i}", [128, ST]) for i in range(2)]
                    ps1 = es.enter_context(nc.psum_tensor(f"lps1_{st}", [128, 512], F32))
                    ps2 = es.enter_context(nc.psum_tensor(f"lps2_{st}", [128, 512], F32))
                    m2 = [None, None]
                    t1 = None
                    for kc in range(16):
                        b = kc % 2
                        tq = P.op("act", lambda e, kc=kc, b=b: e.activation(out=sq[b][:], in_=xTa[:, kc, :], func=AF.Square), [m2[b]])
                        t1 = P.op("pe", lambda e, kc=kc: e.matmul(ps1[:], lhsT=ones_f, rhs=xTa[:, kc, :], start=(kc == 0), stop=(kc == 15)))
                        m2[b] = P.op("pe", lambda e, kc=kc, b=b: e.matmul(ps2[:], lhsT=ones_f, rhs=sq[b][:], start=(kc == 0), stop=(kc == 15)), [tq])
                    t = P.op("dve", lambda e: e.tensor_scalar_mul(out=mean[:], in0=ps1[:], scalar1=1.0 / D), [t1, m2[1]])
                    t = P.op("dve", lambda e: e.tensor_tensor(out=rstd[:], in0=mean[:], in1=mean[:], op=ALU.mult), [t])
                    t = P.op("dve", lambda e: e.scalar_tensor_tensor(out=rstd[:], in0=ps2[:], scalar=1.0 / D, in1=rstd[:], op0=ALU.mult, op1=ALU.subtract), [t])
                    t = P.op("act", lambda e: e.activation(out=rstd[:], in_=rstd[:], func=AF.Sqrt, bias=EPS, scale=1.0), [t])
                    t = P.op("dve", lambda e: e.reciprocal(out=rstd[:], in_=rstd[:]), [t])
                    dfree = [None, None]
                    for kc in range(16):
                        b = kc % 2
                        ta = P.op("dve", lambda e, kc=kc, b=b: e.tensor_tensor(out=dd[b][:], in0=xTa[:, kc, :], in1=mean[:], op=ALU.subtract), [t, dfree[b]])
                        tb = P.op("dve", lambda e, b=b: e.tensor_tensor(out=dd[b][:], in0=dd[b][:], in1=rstd[:], op=ALU.mult), [ta])
                        tc1 = P.op("act", lambda e, kc=kc, b=b: e.activation(out=xTa[:, kc, :], in_=dd[b][:], func=AF.Identity, scale=G1A[:, kc:kc + 1], bias=B1A[:, kc:kc + 1]), [tb])
                        tc2 = P.op("act", lambda e, kc=kc, b=b: e.activation(out=h2T(kc), in_=dd[b][:], func=AF.Identity, scale=A2[:, kc:kc + 1], bias=B2[:, kc:kc + 1]), [tb])
                        dfree[b] = [tc1, tc2]
                        P.op("pool", lambda e, kc=kc: e.tensor_copy(out=h2Tb[:, kc, :], in_=h2T(kc)), [tc2])
                    P.run()

                if stage == 1:
                    break

                with ExitStack() as es:
                  if 'router' not in DEBUG.get('skip', ()):
                      P = Prog(nc, es)
                      def sb(name, shape, dt=F32):
                          return es.enter_context(nc.sbuf_tensor(f"{name}_{st}", list(shape), dt))
                      sc = [sb(f"sc{i}", [128, NE]) for i in range(2)]
                      sel = [sb(f"sel{i}", [128, NE]) for i in range(2)]
                      msk = [sb(f"msk{i}", [128, NE]) for i in range(2)]
                      Mt = [sb(f"Mt{i}", [128, NE]) for i in range(2)]
                      Wn = [sb(f"Wn{i}", [128, NE]) for i in range(2)]
                      pM = [sb(f"pM{i}", [128, NE]) for i in range(2)]
                      t8 = [sb(f"t8{i}", [128, 8, 8]) for i in range(2)]
                      gsc = [sb(f"gsc{i}", [128, 8]) for i in range(2)]
                      g8 = [sb(f"g8{i}", [128, 8]) for i in range(2)]
                      pen = [sb(f"pen{i}", [128, 8]) for i in range(2)]
                      v8 = [sb(f"v8{i}", [128, 8]) for i in range(2)]
                      den = [sb(f"den{i}", [128, 1]) for i in range(2)]
                      h2b = [sb(f"h2b{i}", [128, D], BF16) for i in range(2)]
                      psL = [es.enter_context(nc.psum_tensor(f"psL{i}_{st}", [128, 512], F32)) for i in range(2)]
                      psP = [es.enter_context(nc.psum_tensor(f"psP{i}_{st}", [128, 512], F32)) for i in range(2)]
                      psH = es.enter_context(nc.psum_tensor(f"psH_{st}", [128, D], BF16))
                      so = [P.dsem() for _ in range(7)]
                      wr_sb = sb("wr_sb", [128, 16, NE])
                      t_wr = P.dma("sp", lambda e: e.dma_start(out=wr_sb[:], in_=w_router.rearrange("(kc p) e -> p kc e", p=128)), so[6])
                      last = {}
                      t_srun = None
                      for j in range(4):
                          b = j % 2
                          ti = st * 4 + j
                          tl = None
                          for kc in range(16):
                              tl = P.op("pe", lambda e, kc=kc, j=j, b=b: e.matmul(psL[b][:, 0:NE], lhsT=h2T(kc)[:, j * 128:(j + 1) * 128], rhs=wr_sb[:, kc, :], start=(kc == 0), stop=(kc == 15)),
                                        [last.get(("psL", b)), t_wr] if kc == 0 else [])
                          t = P.op("act", lambda e, b=b: e.activation(out=sc[b][:], in_=psL[b][:, 0:NE], func=AF.Sigmoid), [tl, last.get(("sc", b))])
                          last[("psL", b)] = t
                          t = P.op("dve", lambda e, b=b: e.tensor_tensor(out=sel[b][:], in0=sc[b][:], in1=brep[:], op=ALU.add), [t, last.get(("sel", b))])
                          for g in range(8):
                              t = P.op("dve", lambda e, b=b, g=g: e.max(out=t8[b][:, g, :], in_=sel[b][:, g * 32:(g + 1) * 32]), [t])
                          t = P.op("dve", lambda e, b=b: e.tensor_tensor(out=gsc[b][:], in0=t8[b][:, :, 0], in1=t8[b][:, :, 1], op=ALU.add), [t])
                          t = P.op("dve", lambda e, b=b: e.max(out=g8[b][:], in_=gsc[b][:]), [t])
                          t = P.op("dve", lambda e, b=b: e.tensor_scalar(out=pen[b][:], in0=gsc[b][:], scalar1=g8[b][:, 3:4], scalar2=None, op0=ALU.is_ge), [t])
                          t = P.op("dve", lambda e, b=b: e.tensor_scalar(out=pen[b][:], in0=pen[b][:], scalar1=-1.0, scalar2=BIG, op0=ALU.add, op1=ALU.mult), [t])
                          t = P.op("dve", lambda e, b=b: e.tensor_tensor(out=msk[b][:].rearrange("p (g c) -> p g c", g=8), in0=sel[b][:].rearrange("p (g c) -> p g c", g=8),
                                                                       in1=pen[b][:].unsqueeze(2).broadcast_to([128, 8, 32]), op=ALU.add), [t])
                          t = P.op("dve", lambda e, b=b: e.max(out=v8[b][:], in_=msk[b][:]), [t])
                          tM = P.op("dve", lambda e, b=b: e.tensor_scalar(out=Mt[b][:], in0=msk[b][:], scalar1=v8[b][:, 7:8], scalar2=None, op0=ALU.is_ge), [t, last.get(("Mt", b))])
                          last[("sel", b)] = tM
                          t = P.op("dve", lambda e, b=b: e.tensor_tensor(out=Wn[b][:], in0=Mt[b][:], in1=sc[b][:], op=ALU.mult), [tM, last.get(("Wn", b))])
                          last[("sc", b)] = t
                          t = P.op("dve", lambda e, b=b: e.reduce_sum(out=den[b][:], in_=Wn[b][:], axis=AX.X), [t])
                          t = P.op("dve", lambda e, b=b: e.reciprocal(out=den[b][:], in_=den[b][:]), [t])
                          tW = P.op("dve", lambda e, b=b: e.tensor_scalar(out=Wn[b][:], in0=Wn[b][:], scalar1=den[b][:, 0:1], scalar2=2.5, op0=ALU.mult, op1=ALU.mult), [t])
                          last[("Wn", b)] = P.dma("sp", lambda e, b=b, ti=ti: e.dma_start(out=wn_d[ti], in_=Wn[b][:]), so[b], deps=[tW])
                          tp1 = P.op("pe", lambda e, b=b: e.matmul(psP[b][:, 0:NE], lhsT=ones_f, rhs=Srun[:], start=True, stop=False), [t_srun, last.get(("psP", b))])
                          tp2 = P.op("pe", lambda e, b=b: e.matmul(psP[b][:, 0:NE], lhsT=triS, rhs=Mt[b][:], start=False, stop=True), [tM])
                          t_srun = P.op("pool", lambda e, b=b: e.tensor_tensor(out=Srun[:], in0=Srun[:], in1=Mt[b][:], op=ALU.add), [tp2, tM])
                          tpm = P.op("dve", lambda e, b=b: e.scalar_tensor_tensor(out=pM[b][:], in0=psP[b][:, 0:NE], scalar=1.0, in1=Mt[b][:], op0=ALU.add, op1=ALU.mult), [tp2, last.get(("pM", b))])
                          last[("psP", b)] = tpm
                          last[("pM", b)] = P.dma("sp", lambda e, b=b, ti=ti: e.dma_start(out=posM_d[ti], in_=pM[b][:]), so[2 + b], deps=[tpm])
                          last[("Mt", b)] = [tpm, t_srun]
                          tt = None
                          for kc in range(16):
                              tt = P.op("pe", lambda e, kc=kc, j=j: e.transpose(psH[:, kc * 128:(kc + 1) * 128], h2Tb[:, kc, j * 128:(j + 1) * 128], ident_b[:]),
                                        [last.get("psH")] if kc == 0 else [])
                          tcp = P.op("act", lambda e, b=b: e.activation(out=h2b[b][:], in_=psH[:], func=AF.Identity), [tt, last.get(("h2b", b))])
                          last["psH"] = tcp
                          last[("h2b", b)] = P.dma("sp", lambda e, b=b, ti=ti: e.dma_start(out=h2_d[ti * 128:(ti + 1) * 128, :], in_=h2b[b][:]), so[4 + b], deps=[tcp])
                      P.run()

                with ExitStack() as es2:
                    sg = [es2.enter_context(nc.sbuf_tensor(f"sg{i}_{st}", [128, ST], F32)) for i in range(2)]
                    sgl = [None, None]

                    def evac_sh(P, c, ps, mm):
                        b = c % 2
                        t1 = P.op("act", lambda e: e.activation(out=sg[b][:], in_=ps[0], func=AF.Silu), [mm[0], sgl[b]])
                        t2 = P.op("dve", lambda e: e.tensor_tensor(out=AshT[:, c, :], in0=ps[1], in1=sg[b][:], op=ALU.mult), [mm[1], t1])
                        sgl[b] = t2
                        return [t1, t2]
                    linear_block(nc, f"shg{st}", [(lambda kc: h2Tb[:, kc, :], ws_gate, 0, 16), (lambda kc: h2Tb[:, kc, :], ws_up, 0, 16)], 4, ST, evac_sh, G=2)

                def evac_sd(P, c, ps, mm):
                    t = P.op("dve", lambda e: e.scalar_tensor_tensor(out=xTa[:, c, :], in0=ps[0], scalar=g2p[:, c:c + 1], in1=xTa[:, c, :], op0=ALU.mult, op1=ALU.add), [mm[0]])
                    return [t]
                linear_block(nc, f"shd{st}", [(lambda kc: AshT[:, kc, :], ws_down, 0, 4)], 16, ST, evac_sd, G=4)

                emit_tout(nc, xTa, xr_d, t0, ident_f, f"to{st}")

            if stage == 1:
                emit_tout(nc, xTa, out, 0, ident_f, "todbg")
                return nc
        if stage == 2:
            with ExitStack() as es:
                P = Prog(nc, es)
                bt = [es.enter_context(nc.sbuf_tensor(f"cpb{i}", [128, D], F32)) for i in range(2)]
                s_in = [P.dsem() for _ in range(2)]
                s_out = [P.dsem() for _ in range(2)]
                lo_ = [None, None]
                for ti in range(NTILE):
                    b = ti % 2
                    a = P.dma("sp", lambda e, ti=ti, b=b: e.dma_start(out=bt[b][:], in_=xr_d[ti * 128:(ti + 1) * 128, :]), s_in[b], deps=[lo_[b]])
                    lo_[b] = P.dma("sp", lambda e, ti=ti, b=b: e.dma_start(out=out[ti * 128:(ti + 1) * 128, :], in_=bt[b][:]), s_out[b], deps=[a])
                P.run()
            return nc

        emit_moe(nc, locals())
    return nc


def emit_tout(nc, srcT, dst_d, t0, ident_f, name):
    with ExitStack() as es:
        P = Prog(nc, es)
        ob = [es.enter_context(nc.sbuf_tensor(f"{name}ob{i}", [128, D], F32)) for i in range(2)]
        pst = [es.enter_context(nc.psum_tensor(f"{name}ps{i}", [128, 512], F32)) for i in range(8)]
        so = [P.dsem() for _ in range(2)]
        ps_free = [None] * 8
        ob_free = [None, None]
        for j in range(4):
            b = j % 2
            cps = []
            for q in range(4):
                bank = b * 4 + q
                tp = None
                for k4 in range(4):
                    kc = q * 4 + k4
                    tp = P.op("pe", lambda e, bank=bank, k4=k4, kc=kc, j=j: e.transpose(pst[bank][:, k4 * 128:(k4 + 1) * 128], srcT[:, kc, j * 128:(j + 1) * 128], ident_f),
                              [ps_free[bank]] if k4 == 0 else [])
                eng = "act" if q % 2 == 0 else "dve"
                if eng == "act":
                    tcp = P.op("act", lambda e, bank=bank, q=q, b=b: e.activation(out=ob[b][:, q * 512:(q + 1) * 512], in_=pst[bank][:], func=AF.Identity), [tp, ob_free[b]])
                else:
                    tcp = P.op("dve", lambda e, bank=bank, q=q, b=b: e.tensor_copy(out=ob[b][:, q * 512:(q + 1) * 512], in_=pst[bank][:]), [tp, ob_free[b]])
                ps_free[bank] = tcp
                cps.append(tcp)
            ob_free[b] = P.dma("sp", lambda e, j=j, b=b: e.dma_start(out=dst_d[t0 + j * 128:t0 + (j + 1) * 128, :], in_=ob[b][:]), so[b], deps=cps)
        P.run()


def emit_moe(nc, L):
    import types
    V = types.SimpleNamespace(**L)
    ones_f, triS, triI, ident_f, ident_b = V.ones_f, V.triS, V.triI, V.ident_f, V.ident_b
    Srun, slots_all, W8_all, idx_all, pcol, blkflag = V.Srun, V.slots_all, V.W8_all, V.idx_all, V.pcol, V.blkflag
    xs_d, ys_d, xr_d, h2_d, posM_d, wn_d, out = V.xs_d, V.ys_d, V.xr_d, V.h2_d, V.posM_d, V.wn_d, V.out

    with ExitStack() as gs2:
        pstart = gs2.enter_context(nc.sbuf_tensor("pstart", [128, NE], F32))
        with ExitStack() as es:
            P = Prog(nc, es)
            def sb(name, shape, dt=F32):
                return es.enter_context(nc.sbuf_tensor("m0" + name, list(shape), dt))
            cnt_pp = sb("cnt", [128, 2]); nb_pp = sb("nb", [128, 2]); pad_pp = sb("pad", [128, 2]); pend_pp = sb("pend", [128, 2])
            padbc = sb("padbc", [128, 2, 128]); thr_sb = sb("thr", [128, 512]); ind = [sb(f"ind{i}", [128, 512]) for i in range(2)]
            psC = es.enter_context(nc.psum_tensor("m0psC", [128, 512], F32))
            psS = es.enter_context(nc.psum_tensor("m0psS", [128, 512], F32))
            psE = es.enter_context(nc.psum_tensor("m0psE", [128, 512], F32))
            psB = es.enter_context(nc.psum_tensor("m0psB", [128, 512], F32))
            sl = P.dsem()
            lt = P.dma("sp", lambda e: e.dma_start(out=thr_sb[:], in_=V.thr_d), sl)
            t = None
            for c in range(2):
                t = P.op("pe", lambda e, c=c: e.matmul(psC[:, c:c + 1], lhsT=Srun[:, c * 128:(c + 1) * 128], rhs=ones_f[:, 0:1], start=True, stop=True), [t])
            t = P.op("dve", lambda e: e.tensor_copy(out=cnt_pp[:], in_=psC[:, 0:2]), [t])
            t = P.op("dve", lambda e: e.memset(nb_pp[:], 0.0), [t])
            for m in range(32):
                t = P.op("dve", lambda e, m=m: e.scalar_tensor_tensor(out=nb_pp[:], in0=cnt_pp[:], scalar=128.0 * m, in1=nb_pp[:], op0=ALU.is_gt, op1=ALU.add), [t])
            t = P.op("dve", lambda e: e.tensor_scalar_mul(out=pad_pp[:], in0=nb_pp[:], scalar1=128.0), [t])
            for c in range(2):
                t = P.op("dve", lambda e, c=c: e.tensor_copy(out=padbc[:, c, :], in_=pad_pp[:, c:c + 1].broadcast_to([128, 128])), [t])
            tp = P.op("pe", lambda e: e.matmul(psS[:, 0:128], lhsT=padbc[:, 0, :], rhs=triS, start=True, stop=True), [t])
            tp = P.op("pe", lambda e: e.matmul(psS[:, 128:256], lhsT=padbc[:, 0, :], rhs=ones_f, start=True, stop=False), [tp])
            tp = P.op("pe", lambda e: e.matmul(psS[:, 128:256], lhsT=padbc[:, 1, :], rhs=triS, start=False, stop=True), [tp])
            t = P.op("dve", lambda e: e.tensor_copy(out=pstart[:], in_=psS[:, 0:NE]), [tp])
            tp = P.op("pe", lambda e: e.matmul(psE[:, 0:1], lhsT=triI, rhs=pad_pp[:, 0:1], start=True, stop=True), [tp])
            tp = P.op("pe", lambda e: e.matmul(psE[:, 1:2], lhsT=ones_f, rhs=pad_pp[:, 0:1], start=True, stop=False), [tp])
            tp = P.op("pe", lambda e: e.matmul(psE[:, 1:2], lhsT=triI, rhs=pad_pp[:, 1:2], start=False, stop=True), [tp])
            t = P.op("dve", lambda e: e.tensor_copy(out=pend_pp[:], in_=psE[:, 0:2]), [tp, t])
            for c in range(2):
                t = P.op("dve", lambda e, c=c: e.tensor_scalar(out=ind[c][:], in0=thr_sb[:], scalar1=pend_pp[:, c:c + 1], scalar2=None, op0=ALU.is_ge), [t, lt])
            for c in range(2):
                tp = P.op("pe", lambda e, c=c: e.matmul(psB[:, :], lhsT=ones_f, rhs=ind[c][:], start=(c == 0), stop=(c == 1)), [t, tp])
            def _mk_bnd(e):
                POOL["regs"]["b"] = e.alloc_register("bndreg")
                e.reg_mov(POOL["regs"]["b"], NE * 512 - 1)
            P.raw("pool", _mk_bnd)
            pc4 = sb("pc4", [128, 4])
            ncb_all = NBLK // BPB
            for cbi in range(ncb_all):
                b0 = cbi * BPB
                t = P.op("dve", lambda e, cbi=cbi, b0=b0: e.tensor_scalar(out=blkflag[0:1, cbi:cbi + 1], in0=psB[0:1, b0:b0 + 1], scalar1=255.5, scalar2=None, op0=ALU.is_lt), [tp, t])
            for q in range(4):
                t = P.op("dve", lambda e, q=q: e.tensor_scalar(out=pc4[:, q:q + 1], in0=pcol, scalar1=4.0, scalar2=float(q), op0=ALU.mult, op1=ALU.add), [t])
            for q in range(4):
                t = P.op("dve", lambda e, q=q: e.tensor_scalar(out=idx_all[:, q, :], in0=psB[:, :], scalar1=512.0, scalar2=pc4[:, q:q + 1], op0=ALU.mult, op1=ALU.add), [tp, t])
            P.run()

        for half in range(4):
            with ExitStack() as es:
                P = Prog(nc, es)
                def sb(name, shape, dt=F32):
                    return es.enter_context(nc.sbuf_tensor(f"dp{half}{name}", list(shape), dt))
                pM = [sb(f"pM{i}", [128, NE]) for i in range(2)]
                Wn = [sb(f"Wn{i}", [128, NE]) for i in range(2)]
                hb = [sb(f"hb{i}", [128, D], BF16) for i in range(2)]
                key = [sb(f"key{i}", [128, NE]) for i in range(2)]
                mk = sb("mk", [128, NE]); junk = sb("junk", [128, NE]); s8 = [sb(f"s8{i}", [128, 8]) for i in range(2)]
                sl = [P.dsem() for _ in range(6)]
                ssc = [P.dsem() for _ in range(2)]
                free = {}
                for ii in range(8):
                    i = half * 8 + ii
                    b = ii % 2
                    l1 = P.dma("sp", lambda e, i=i, b=b: e.dma_start(out=pM[b][:], in_=posM_d[i]), sl[b], deps=[free.get(("pM", b))])
                    l2 = P.dma("sp", lambda e, i=i, b=b: e.dma_start(out=Wn[b][:], in_=wn_d[i]), sl[2 + b], deps=[free.get(("Wn", b))])
                    l3 = P.dma("sp", lambda e, i=i, b=b: e.dma_start(out=hb[b][:], in_=h2_d[i * 128:(i + 1) * 128, :]), sl[4 + b], deps=[free.get(("hb", b))])
                    t = P.op("dve", lambda e, b=b: e.tensor_scalar(out=mk[:], in0=pM[b][:], scalar1=0.5, scalar2=None, op0=ALU.is_gt), [l1])
                    t = P.op("dve", lambda e, b=b: e.tensor_tensor(out=key[b][:], in0=pM[b][:], in1=pstart[:], op=ALU.add), [t, free.get(("key", b))])
                    free[("pM", b)] = t
                    t = P.op("dve", lambda e, b=b: e.tensor_tensor(out=key[b][:], in0=key[b][:], in1=mk[:], op=ALU.mult), [t])
                    t = P.op("dve", lambda e, b=b: e.max(out=s8[b][:], in_=key[b][:]), [t, free.get(("s8", b))])
                    tsl = P.op("dve", lambda e, b=b, i=i: e.tensor_scalar(out=slots_all[:, i, :], in0=s8[b][:], scalar1=-1.0, scalar2=None, op0=ALU.add), [t])
                    for k in range(8):
                        t = P.op("dve", lambda e, b=b, i=i, k=k: e.scalar_tensor_tensor(out=junk[:], in0=key[b][:], scalar=s8[b][:, k:k + 1], in1=Wn[b][:], op0=ALU.is_equal, op1=ALU.mult,
                                                                                    accum_out=W8_all[:, i, k:k + 1]), [t, l2])
                    free[("Wn", b)] = t
                    free[("key", b)] = t
                    free[("s8", b)] = t
                    sc_t = None
                    for k in range(8):
                        sc_t = P.dma("pool", lambda e, b=b, i=i, k=k: e.indirect_dma_start(out=xs_d[:, :], out_offset=bass.IndirectOffsetOnAxis(ap=slots_all[:, i, k:k + 1], axis=0),
                                                                                      in_=hb[b][:, :], in_offset=None), ssc[b], deps=[tsl, l3])
                    free[("hb", b)] = sc_t
                P.run()

        ncb_all = NBLK // BPB
        for cb in range(0 if 'experts' in DEBUG.get('skip', ()) else DEBUG.get('ncb', ncb_all)):
            with ExitStack() as es:
                ci = cb - (ncb_all - NCOND)
                if ci >= 0 and not DEBUG.get('nocond'):
                    P = Prog(nc, es, cond=blkflag[0:1, cb:cb + 1], dedicated=POOL["ded"][ci])
                else:
                    P = Prog(nc, es)
                def sb(name, shape, dt=F32):
                    return es.enter_context(nc.sbuf_tensor(f"mx{cb}{name}", list(shape), dt))
                wg = [sb(f"wg{i}", [128, 16, 512], BF16) for i in range(2)]
                wu = [sb(f"wu{i}", [128, 16, 512], BF16) for i in range(2)]
                wd = [sb(f"wd{i}", [128, 4, D], BF16) for i in range(2)]
                NSTG = 6
                stg = [sb(f"stg{i}", [128, 2048]) for i in range(NSTG)]
                Xg = [sb(f"Xg{i}", [128, D], BF16) for i in range(2)]
                XgT = [sb(f"XgT{i}", [128, 16, 128], BF16) for i in range(2)]
                sgt = sb("sgt", [128, 512]); Ab = sb("Ab", [128, 512], BF16); AT = sb("AT", [128, 4, 128], BF16)
                Yb = [sb(f"Yb{i}", [128, D], BF16) for i in range(2)]
                psX = es.enter_context(nc.psum_tensor(f"mx{cb}psX", [128, D], BF16))
                psG = es.enter_context(nc.psum_tensor(f"mx{cb}psG", [128, 512], F32))
                psU = es.enter_context(nc.psum_tensor(f"mx{cb}psU", [128, 512], F32))
                psA = es.enter_context(nc.psum_tensor(f"mx{cb}psA", [128, 512], BF16))
                psY = [es.enter_context(nc.psum_tensor(f"mx{cb}psY{i}", [128, 512], F32)) for i in range(3)]
                sstg = [P.dsem() for _ in range(NSTG)]
                sxg = [P.dsem() for _ in range(2)]; syb = [P.dsem() for _ in range(2)]
                fr = {}
                regs = POOL["regs"]
                wgv = V.w_gate.rearrange("e (p q kk) f -> (e p q) (kk f)", q=4, kk=4)
                wuv = V.w_up.rearrange("e (p q kk) f -> (e p q) (kk f)", q=4, kk=4)
                wdv = V.w_down.rearrange("e (p q) d -> (e p q) d", q=4)
                order = [("g", 0), ("u", 0), ("g", 1), ("u", 1), ("g", 2), ("u", 2), ("g", 3), ("u", 3), ("d", 0), ("d", 1), ("d", 2), ("d", 3)]
                total = BPB * 12
                gt = {}
                ct = {}
                stg_free = [None] * NSTG
                sp_ = {"g": 0, "c": 0}

                def emit_gather(i):
                    bl, c = divmod(i, 12)
                    bg = cb * BPB + bl
                    kind, q = order[c]
                    slot = i % NSTG
                    src = {"g": wgv, "u": wuv, "d": wdv}[kind]

                    def f(e, slot=slot, src=src, bg=bg, q=q):
                        return e.indirect_dma_start(out=stg[slot][:, :], out_offset=None, in_=src, in_offset=bass.IndirectOffsetOnAxis(ap=idx_all[:, q, bg:bg + 1], axis=0),
                                                    bounds_check=regs["b"], oob_is_err=False)
                    gt[i] = P.dma("pool", f, sstg[slot], deps=[stg_free[slot]])

                def emit_cast(i):
                    bl, c = divmod(i, 12)
                    s = bl % 2
                    kind, q = order[c]
                    slot = i % NSTG
                    if kind == "g":
                        t = P.op("act", lambda e, s=s, q=q, slot=slot: e.activation(out=wg[s][:, 4 * q:4 * q + 4, :], in_=stg[slot][:].rearrange("p (k f) -> p k f", k=4), func=AF.Identity),
                                 [gt[i], fr.get(("wgu", s))])
                    elif kind == "u":
                        t = P.op("dve", lambda e, s=s, q=q, slot=slot: e.tensor_copy(out=wu[s][:, 4 * q:4 * q + 4, :], in_=stg[slot][:].rearrange("p (k f) -> p k f", k=4)),
                                 [gt[i], fr.get(("wgu", s))])
                    elif q % 2 == 0:
                        t = P.op("dve", lambda e, s=s, q=q, slot=slot: e.tensor_copy(out=wd[s][:, q, :], in_=stg[slot][:]), [gt[i], fr.get(("wd", s))])
                    else:
                        t = P.op("act", lambda e, s=s, q=q, slot=slot: e.activation(out=wd[s][:, q, :], in_=stg[slot][:], func=AF.Identity), [gt[i], fr.get(("wd", s))])
                    stg_free[slot] = t
                    ct[i] = t

                def stream_to_cast(c_end):
                    c_end = min(c_end, total)
                    while sp_["c"] < c_end:
                        while sp_["g"] < min(sp_["c"] + NSTG, total):
                            emit_gather(sp_["g"])
                            sp_["g"] += 1
                        emit_cast(sp_["c"])
                        sp_["c"] += 1

                xl = {}

                def issue_x(bl):
                    bg = cb * BPB + bl
                    s = bl % 2
                    xl[bl] = P.dma("sp", lambda e, bg=bg, s=s: e.dma_start(out=Xg[s][:], in_=xs_d[bg * 128:(bg + 1) * 128, :]), sxg[s], deps=[fr.get(("Xg", s))])

                t1c = {}

                def emit_T1(bl):
                    s = bl % 2
                    tp = None
                    for kc in range(16):
                        tp = P.op("pe", lambda e, kc=kc, s=s: e.transpose(psX[:, kc * 128:(kc + 1) * 128], Xg[s][:].rearrange("s (p k) -> s k p", k=16)[:, kc, :], ident_b[:]),
                                  [xl[bl], fr.get("psX")] if kc == 0 else [])
                    fr[("Xg", s)] = tp
                    c1 = P.op("act", lambda e, s=s: e.activation(out=XgT[s][:, 0:8, :], in_=psX[:, 0:1024].rearrange("p (a c) -> p a c", a=8), func=AF.Identity), [tp, fr.get(("XgT", s))])
                    c2 = P.op("dve", lambda e, s=s: e.tensor_copy(out=XgT[s][:, 8:16, :], in_=psX[:, 1024:2048].rearrange("p (a c) -> p a c", a=8)), [tp, fr.get(("XgT", s))])
                    fr["psX"] = [c1, c2]
                    t1c[bl] = [c1, c2]

                issue_x(0)
                issue_x(1)
                emit_T1(0)
                for bl in range(BPB):
                    bg = cb * BPB + bl
                    s = bl % 2
                    stream_to_cast(bl * 12 + 8)
                    tg = None
                    for kc in range(16):
                        d0 = [[ct[bl * 12 + c] for c in range(8)], t1c[bl], fr.get("psGU")] if kc == 0 else []
                        P.op("pe", lambda e, kc=kc, s=s: e.matmul(psG[:], lhsT=XgT[s][:, kc, :], rhs=wg[s][:, kc, :], start=(kc == 0), stop=(kc == 15)), d0)
                        tg = P.op("pe", lambda e, kc=kc, s=s: e.matmul(psU[:], lhsT=XgT[s][:, kc, :], rhs=wu[s][:, kc, :], start=(kc == 0), stop=(kc == 15)))
                    fr[("wgu", s)] = tg
                    stream_to_cast(bl * 12 + 12)
                    fr[("XgT", s)] = tg
                    if bl + 1 < BPB:
                        emit_T1(bl + 1)
                    ta = P.op("act", lambda e: e.activation(out=sgt[:], in_=psG[:], func=AF.Silu), [tg, fr.get("sgt")])
                    tb = P.op("dve", lambda e: e.tensor_tensor(out=Ab[:], in0=psU[:], in1=sgt[:], op=ALU.mult), [ta, fr.get("Ab")])
                    fr["sgt"] = tb
                    fr["psGU"] = tb
                    tp = None
                    for fc in range(4):
                        tp = P.op("pe", lambda e, fc=fc: e.transpose(psA[:, fc * 128:(fc + 1) * 128], Ab[:].rearrange("s (p k) -> s k p", k=4)[:, fc, :], ident_b[:]), [tb, fr.get("psA")] if fc == 0 else [])
                    fr["Ab"] = tp
                    tat = P.op("act", lambda e: e.activation(out=AT[:], in_=psA[:].rearrange("p (a c) -> p a c", a=4), func=AF.Identity), [tp, fr.get("AT")])
                    fr["psA"] = tat
                    ycp = []
                    ty = None
                    for q in range(4):
                        pq = q % 3
                        for fc in range(4):
                            d0 = [tat, [ct[bl * 12 + 8 + c] for c in range(4)], fr.get(("psY", pq))] if fc == 0 else []
                            ty = P.op("pe", lambda e, fc=fc, q=q, pq=pq, s=s: e.matmul(psY[pq][:], lhsT=AT[:, fc, :], rhs=wd[s][:, fc, q * 512:(q + 1) * 512], start=(fc == 0), stop=(fc == 3)), d0)
                        if q % 2 == 0:
                            tc_ = P.op("act", lambda e, q=q, pq=pq, s=s: e.activation(out=Yb[s][:, q * 512:(q + 1) * 512], in_=psY[pq][:], func=AF.Identity), [ty, fr.get(("Yb", s))])
                        else:
                            tc_ = P.op("dve", lambda e, q=q, pq=pq, s=s: e.tensor_copy(out=Yb[s][:, q * 512:(q + 1) * 512], in_=psY[pq][:]), [ty, fr.get(("Yb", s))])
                        fr[("psY", pq)] = tc_
                        ycp.append(tc_)
                    fr["AT"] = ty
                    fr[("wd", s)] = ty
                    fr[("Yb", s)] = P.dma("sp", lambda e, bg=bg, s=s: e.dma_start(out=ys_d[bg * 128:(bg + 1) * 128, :], in_=Yb[s][:]), syb[s], deps=ycp)
                    if bl + 2 < BPB:
                        issue_x(bl + 2)
                P.run()

        for part in range(0 if 'combine' in DEBUG.get('skip', ()) else 4):
            with ExitStack() as es:
                P = Prog(nc, es)
                def sb(name, shape, dt=F32):
                    return es.enter_context(nc.sbuf_tensor(f"cm{part}{name}", list(shape), dt))
                g2rep = sb("g2rep", [128, D]); ln2g = sb("ln2g", [128, D]); ln2b = sb("ln2b", [128, D])
                rk = [sb(f"rk{i}", [128, 128]) for i in range(2)]
                Yg = [[sb(f"Yg{i}_{k}", [128, D], BF16) for k in range(8)] for i in range(2)]
                xr = [sb(f"xr{i}", [128, D]) for i in range(2)]
                acc = sb("acc", [128, D]); sqj = sb("sqj", [128, D]); ob = [sb(f"ob{i}", [128, D]) for i in range(2)]
                st4 = [sb(f"st{i}", [128, 4]) for i in range(2)]
                psg = [es.enter_context(nc.psum_tensor(f"cm{part}psg{i}", [128, 512], F32)) for i in range(4)]
                sl = P.dsem()
                sgk = [[P.dsem() for k in range(8)] for i in range(2)]
                sxr = [P.dsem() for _ in range(2)]; sob = [P.dsem() for _ in range(2)]
                P.dma("sp", lambda e: e.dma_start(out=ln2g[:], in_=V.ln2g_d), sl)
                lt = P.dma("sp", lambda e: e.dma_start(out=ln2b[:], in_=V.ln2b_d), sl)
                mmt = [None, None]
                t = None
                for kc in range(16):
                    t = P.op("dve", lambda e, kc=kc: e.tensor_scalar_mul(out=rk[kc % 2][:], in0=ident_f, scalar1=V.g2p[:, kc:kc + 1]), [t, mmt[kc % 2]])
                    mmt[kc % 2] = P.op("pe", lambda e, kc=kc: e.matmul(psg[kc // 4][:, (kc % 4) * 128:(kc % 4 + 1) * 128], lhsT=ones_f, rhs=rk[kc % 2][:], start=True, stop=True), [t])
                for q in range(4):
                    t = P.op("dve", lambda e, q=q: e.tensor_copy(out=g2rep[:, q * 512:(q + 1) * 512], in_=psg[q][:]), [t, mmt[0], mmt[1]])
                fr = {}
                for ii in range(8):
                    i = part * 8 + ii
                    b = ii % 2
                    gt = []
                    for k in range(8):
                        gt.append(P.dma("pool", lambda e, b=b, i=i, k=k: e.indirect_dma_start(out=Yg[b][k][:, :], out_offset=None, in_=ys_d[:, :],
                                                                                         in_offset=bass.IndirectOffsetOnAxis(ap=slots_all[:, i, k:k + 1], axis=0)), sgk[b][k], deps=[fr.get(("Yg", b))]))
                    lx = P.dma("sp", lambda e, b=b, i=i: e.dma_start(out=xr[b][:], in_=xr_d[i * 128:(i + 1) * 128, :]), sxr[b], deps=[fr.get(("xr", b))])
                    t = P.op("dve", lambda e, b=b, i=i: e.tensor_scalar_mul(out=acc[:], in0=Yg[b][0][:], scalar1=W8_all[:, i, 0:1]), [gt[0], t, fr.get("acc")])
                    for k in range(1, 8):
                        t = P.op("dve", lambda e, b=b, i=i, k=k: e.scalar_tensor_tensor(out=acc[:], in0=Yg[b][k][:], scalar=W8_all[:, i, k:k + 1], in1=acc[:], op0=ALU.mult, op1=ALU.add), [gt[k], t])
                    fr[("Yg", b)] = t
                    t = P.op("dve", lambda e: e.tensor_tensor(out=acc[:], in0=acc[:], in1=g2rep[:], op=ALU.mult), [t])
                    t = P.op("dve", lambda e, b=b: e.tensor_tensor(out=acc[:], in0=acc[:], in1=xr[b][:], op=ALU.add), [t, lx])
                    fr[("xr", b)] = t
                    ta1 = P.op("act", lambda e, b=b: e.activation(out=sqj[:], in_=acc[:], func=AF.Identity, accum_out=st4[b][:, 0:1]), [t, fr.get("sqj"), fr.get(("st", b))])
                    ta2 = P.op("act", lambda e, b=b: e.activation(out=sqj[:], in_=acc[:], func=AF.Square, accum_out=st4[b][:, 1:2]), [ta1])
                    t = P.op("dve", lambda e, b=b: e.tensor_scalar_mul(out=st4[b][:, 0:2], in0=st4[b][:, 0:2], scalar1=1.0 / D), [ta2])
                    t = P.op("dve", lambda e, b=b: e.tensor_tensor(out=st4[b][:, 2:3], in0=st4[b][:, 0:1], in1=st4[b][:, 0:1], op=ALU.mult), [t])
                    t = P.op("dve", lambda e, b=b: e.tensor_tensor(out=st4[b][:, 2:3], in0=st4[b][:, 1:2], in1=st4[b][:, 2:3], op=ALU.subtract), [t])
                    ts = P.op("act", lambda e, b=b: e.activation(out=st4[b][:, 3:4], in_=st4[b][:, 2:3], func=AF.Sqrt, bias=EPS, scale=1.0), [t])
                    fr["sqj"] = ts
                    t = P.op("dve", lambda e, b=b: e.reciprocal(out=st4[b][:, 3:4], in_=st4[b][:, 3:4]), [ts])
                    t = P.op("dve", lambda e, b=b: e.tensor_scalar(out=ob[b][:], in0=acc[:], scalar1=st4[b][:, 0:1], scalar2=st4[b][:, 3:4], op0=ALU.subtract, op1=ALU.mult), [t, fr.get(("ob", b))])
                    fr["acc"] = t
                    fr[("st", b)] = t
                    tq = P.op("pool", lambda e, b=b: e.tensor_tensor(out=ob[b][:], in0=ob[b][:], in1=ln2g[:], op=ALU.mult), [t, lt])
                    tq = P.op("pool", lambda e, b=b: e.tensor_tensor(out=ob[b][:], in0=ob[b][:], in1=ln2b[:], op=ALU.add), [tq])
                    fr[("ob", b)] = P.dma("sp", lambda e, b=b, i=i: e.dma_start(out=out[i * 128:(i + 1) * 128, :], in_=ob[b][:]), sob[b], deps=[tq])
                P.run()


def _pk(v, n):
    return np.ascontiguousarray(np.asarray(v, np.float32).reshape(n, 128).T)


def make_inputs(b, inp):
    f = lambda a: np.ascontiguousarray(np.asarray(a, np.float32))
    m = {}
    m["x"] = f(inp["x"][b])
    m["cp"] = _pk(inp["c"][b], 16)
    m["w_ada"] = f(inp["w_ada"][0])
    m["badap"] = np.ascontiguousarray(np.concatenate([_pk(inp["b_ada"][0][s * D:(s + 1) * D], 16) for s in range(6)], axis=1))
    m["w_in"] = f(inp["w_in"][0])
    m["w_s"] = f(inp["w_s"][0])
    m["bsT"] = np.ascontiguousarray(np.broadcast_to(f(inp["b_s"][0])[None], (128, 8, 128)))
    m["gvp"] = _pk(inp["g_v"][0], 8)
    m["bvp"] = _pk(inp["b_v"][0], 8)
    m["w_pool"] = f(inp["w_pool"][0])
    m["bpp"] = _pk(inp["b_pool"][0], 8)
    m["lsp"] = _pk(inp["ls_pool"][0], 8)
    m["w_pa"] = f(inp["w_pa"][0])
    m["w_pb"] = f(inp["w_pb"][0])
    m["w_o"] = f(inp["w_o"][0])
    m["ln1gp"] = _pk(inp["ln1_g"][0], 16)
    m["ln1bp"] = _pk(inp["ln1_b"][0], 16)
    m["w_router"] = f(inp["w_router"][0])
    m["brep"] = np.ascontiguousarray(np.broadcast_to(f(inp["b_router"][0])[None], (128, NE)))
    m["w_gate"] = f(inp["w_gate"][0])
    m["w_up"] = f(inp["w_up"][0])
    m["w_down"] = f(inp["w_down"][0])
    m["ws_gate"] = f(inp["ws_gate"][0])
    m["ws_up"] = f(inp["ws_up"][0])
    m["ws_down"] = f(inp["ws_down"][0])
    m["ln2grep"] = np.ascontiguousarray(np.broadcast_to(f(inp["ln2_g"][0])[None], (128, D)))
    m["ln2brep"] = np.ascontiguousarray(np.broadcast_to(f(inp["ln2_b"][0])[None], (128, D)))
    cst = np.zeros((128, 5, 128), np.float32)
    cst[:, 0, :] = np.eye(128)
    cst[:, 1, :] = 1.0
    i = np.arange(128)
    cst[:, 2, :] = (i[:, None] < i[None, :])
    cst[:, 3, :] = (i[:, None] <= i[None, :])
    cst[:, 4, :] = i[:, None]
    m["cst"] = cst
    m["thr"] = np.ascontiguousarray(np.broadcast_to((128.0 * np.arange(512, dtype=np.float32))[None], (128, 512)))
    rc = np.zeros((128, 4, 16), np.float32)
    for g in range(4):
        rc[:, g, :] = 1.0 / np.minimum(np.arange(1, 17), 2 << g)
    m["rc"] = rc
    return m


_NC_CACHE = {}


def kernel(**inputs):
    stage = 99
    if stage not in _NC_CACHE:
        _NC_CACHE[stage] = build(stage)
    nc = _NC_CACHE[stage]
    in_maps = [make_inputs(b, inputs) for b in range(8)]
    res = run_bass_kernel_spmd(nc, in_maps, core_ids=list(range(8)))
    return np.stack([np.asarray(r["out"], np.float32) for r in res.results], axis=0)
```
